# Optimizing a Trainium2 kernel written in Bass

```python
import jax, jax.numpy as jnp
from jax import lax
import numpy as np

D_MODEL = 1024
BATCH = 16
SEQ = 2048
DEPTH = 4

GLA_HEADS = 4
GLA_KEY = D_MODEL // 2
GLA_VAL = D_MODEL
GLA_DK = GLA_KEY // GLA_HEADS
GLA_DV = GLA_VAL // GLA_HEADS
GLA_GATE_RANK = 16
GLA_TAU = 16.0
GLA_CHUNK = 64
GLA_SPLITS = (GLA_KEY, GLA_KEY, GLA_VAL, GLA_VAL, GLA_GATE_RANK)
GLA_COLS = 2 * GLA_KEY + 2 * GLA_VAL + GLA_GATE_RANK
RWKV_DIM = D_MODEL // 2
RWKV_HEAD = 64
RWKV_HEADS = RWKV_DIM // RWKV_HEAD
RWKV_DECAY_RANK = 64
RWKV_A_RANK = 64
RWKV_G_RANK = 128
RWKV_SPLITS = (RWKV_DIM, RWKV_DECAY_RANK, RWKV_DIM, RWKV_DIM, RWKV_A_RANK, RWKV_G_RANK)
RWKV_COLS = 3 * RWKV_DIM + RWKV_DECAY_RANK + RWKV_A_RANK + RWKV_G_RANK
RWKV_LN_EPS = 64e-5
N_IN = GLA_COLS + RWKV_COLS + 2 * D_MODEL
FFN_DIM = ((8 * D_MODEL // 3 + 255) // 256) * 256
N_EXPERTS = 8
MOE_TOP_K = 2
MOE_FFN = 7 * D_MODEL // 2
N_DENSE = (DEPTH + 1) // 2
N_MOE = DEPTH // 2
NORM_EPS = 1e-6

kernel_name = 'hybrid_gla_rwkv7_moe_adaln'


def _rmsnorm(x, g):
    x32 = x.astype(jnp.float32)
    y = x32 * lax.rsqrt(jnp.mean(x32 * x32, axis=-1, keepdims=True) + NORM_EPS)
    return (y * g.astype(jnp.float32)).astype(x.dtype)


def _split(p, sizes):
    out = []
    o = 0
    for s in sizes:
        out.append(p[..., o:o + s])
        o += s
    return out


def _token_shift(p):
    return jnp.pad(p, ((0, 0), (1, 0), (0, 0)))[:, :-1, :]


def _gla_chunked(q, k, v, log_a):
    B_, S_, H, DKh = q.shape
    DVh = v.shape[-1]
    C = GLA_CHUNK
    N = S_ // C

    def to_chunks(t):
        return t.reshape(B_, N, C, H, t.shape[-1]).transpose(1, 0, 3, 2, 4)

    q, k, v, log_a = to_chunks(q), to_chunks(k), to_chunks(v), to_chunks(log_a)
    b = jnp.cumsum(log_a, axis=3)
    b_last = b[:, :, :, -1:, :]
    qe = q * jnp.exp(b)
    ke = k * jnp.exp(-b)
    kd = k * jnp.exp(b_last - b)
    causal = jnp.tril(jnp.ones((C, C), dtype=bool))
    att = jnp.where(causal, jnp.einsum('nbhid,nbhjd->nbhij', qe, ke), 0.0)
    o_intra = jnp.einsum('nbhij,nbhjv->nbhiv', att, v)

    def step(state, xs):
        qe_n, kd_n, v_n, dl_n = xs
        o_n = jnp.einsum('bhid,bhdv->bhiv', qe_n, state)
        state = dl_n[..., 0, :, None] * state + jnp.einsum('bhjd,bhjv->bhdv', kd_n, v_n)
        return state, o_n

    state0 = jnp.zeros((B_, H, DKh, DVh), q.dtype)
    _, o_inter = lax.scan(step, state0, (qe, kd, v, jnp.exp(b_last)))
    o = o_intra + o_inter
    return o.transpose(1, 0, 3, 2, 4).reshape(B_, S_, H, DVh)


def _gla_branch(pq, pk, pv, pg, pa, alpha_up, alpha_b, norm_g, w_o):
    B_, S_, _ = pq.shape
    f32 = jnp.float32
    q = pq.astype(f32).reshape(B_, S_, GLA_HEADS, GLA_DK) * (GLA_DK ** -0.5)
    k = pk.astype(f32).reshape(B_, S_, GLA_HEADS, GLA_DK)
    v = pv.astype(f32).reshape(B_, S_, GLA_HEADS, GLA_DV)
    log_a = jax.nn.log_sigmoid((pa.astype(f32) @ alpha_up + alpha_b).astype(f32)) / GLA_TAU
    log_a = log_a.reshape(B_, S_, GLA_HEADS, GLA_DK)
    o = _gla_chunked(q, k, v, log_a)
    o = o * lax.rsqrt(jnp.mean(o * o, axis=-1, keepdims=True) + 1e-5)
    o = o * norm_g.astype(f32).reshape(GLA_HEADS, GLA_DV)
    o = o.reshape(B_, S_, GLA_VAL) * jax.nn.silu(pg.astype(f32))
    return o.astype(pq.dtype) @ w_o


def _rwkv7_scan(r, w, k, v, a, b):
    B_, S_, H, N = r.shape
    xs = tuple(t.transpose(1, 0, 2, 3) for t in (r, w, k, v, a, b))

    def step(state, xt):
        r_t, w_t, k_t, v_t, a_t, b_t = xt
        sa = jnp.einsum('bhvk,bhk->bhv', state, a_t)
        state = (state * w_t[:, :, None, :] + sa[..., None] * b_t[:, :, None, :]
                 + v_t[..., None] * k_t[:, :, None, :])
        return state, jnp.einsum('bhvk,bhk->bhv', state, r_t)

    _, y = lax.scan(step, jnp.zeros((B_, H, N, N), r.dtype), xs)
    return y.transpose(1, 0, 2, 3)


def _rwkv7_branch(p, mu, w0, w2, a0, a2, g2, k_k, k_a, r_k, lnx_g, lnx_b, w_o):
    B_, S_, _ = p.shape
    f32 = jnp.float32
    p32 = p.astype(f32)
    p32 = p32 + (_token_shift(p32) - p32) * mu.astype(f32)
    r, wd, k, v, ad, gd = _split(p32, RWKV_SPLITS)
    w_log = -jax.nn.softplus(-(w0.astype(f32) + jnp.tanh(wd) @ w2.astype(f32))) - 0.5
    decay = jnp.exp(-jnp.exp(w_log))
    a = jax.nn.sigmoid(a0.astype(f32) + ad @ a2.astype(f32))
    g = jax.nn.sigmoid(gd) @ g2.astype(f32)

    def hr(t):
        return t.reshape(B_, S_, RWKV_HEADS, RWKV_HEAD)

    kk = hr(k * k_k.astype(f32))
    kk = kk / jnp.maximum(jnp.sqrt(jnp.sum(kk * kk, axis=-1, keepdims=True)), 1e-12)
    k = k * (1.0 + (a - 1.0) * k_a.astype(f32))
    rh, kh, vh = hr(r), hr(k), hr(v)
    y = _rwkv7_scan(rh, hr(decay), kh, vh, -kk, kk * hr(a))
    mean = jnp.mean(y, axis=-1, keepdims=True)
    var = jnp.mean(jnp.square(y - mean), axis=-1, keepdims=True)
    y = ((y - mean) * lax.rsqrt(var + RWKV_LN_EPS)).reshape(B_, S_, RWKV_DIM)
    y = y * lnx_g.astype(f32) + lnx_b.astype(f32)
    bonus = jnp.sum(rh * kh * r_k.astype(f32), axis=-1, keepdims=True) * vh
    y = (y + bonus.reshape(B_, S_, RWKV_DIM)) * g
    return y.astype(p.dtype) @ w_o


def _mixer(h, w_in, alpha_up, alpha_b, gla_norm_g, gla_w_o, mu, w0, w2, a0, a2, g2,
           k_k, k_a, r_k, lnx_g, lnx_b, rwkv_w_o, w_out):
    p = h @ w_in
    p_gla, p_rwkv, p_gate = _split(p, (GLA_COLS, RWKV_COLS, 2 * D_MODEL))
    pq, pk, pv, pg, pa = _split(p_gla, GLA_SPLITS)
    y_a = _gla_branch(pq, pk, pv, pg, pa, alpha_up, alpha_b, gla_norm_g, gla_w_o)
    y_b = _rwkv7_branch(p_rwkv, mu, w0, w2, a0, a2, g2, k_k, k_a, r_k, lnx_g, lnx_b, rwkv_w_o)
    gate_a, gate_b = _split(p_gate, (D_MODEL, D_MODEL))
    merged = jax.nn.sigmoid(gate_a) * y_a + jax.nn.sigmoid(gate_b) * y_b
    return merged.astype(h.dtype) @ w_out


def _swiglu(h, w1, w3, w2):
    return (jax.nn.silu(h @ w1) * (h @ w3)) @ w2


def _moe(h, router, w1, w3, w2):
    logits = (h @ router).astype(jnp.float32)
    top_v, top_i = lax.top_k(logits, MOE_TOP_K)
    wts = jax.nn.softmax(top_v, axis=-1)
    combine = jnp.sum(jax.nn.one_hot(top_i, N_EXPERTS, dtype=jnp.float32) * wts[..., None], axis=-2)
    combine = combine.astype(h.dtype)
    y = jnp.zeros_like(h)
    for e in range(N_EXPERTS):
        y = y + combine[..., e:e + 1] * _swiglu(h, w1[e], w3[e], w2[e])
    return y


def setup_inputs(seed: int = 0) -> dict:
    key = jax.random.key(seed)
    ks = jax.random.split(key, 32)
    f32 = jnp.float32
    L, D = DEPTH, D_MODEL

    def nrm(k, shape, s):
        return jax.random.normal(k, shape, f32) * s

    return {
        'x': nrm(ks[0], (BATCH, SEQ, D), 1.0),
        'c': nrm(ks[1], (BATCH, D), 1.0),
        'w_ada': nrm(ks[2], (L, D, 6 * D), 0.5 * D ** -0.5),
        'b_ada': nrm(ks[3], (L, 6 * D), 0.01),
        'ln1_g': 1.0 + nrm(ks[4], (L, D), 0.02),
        'ln2_g': 1.0 + nrm(ks[5], (L, D), 0.02),
        'w_in': nrm(ks[6], (L, D, N_IN), D ** -0.5),
        'gla_alpha_up': nrm(ks[7], (L, GLA_GATE_RANK, GLA_KEY), GLA_GATE_RANK ** -0.5),
        'gla_alpha_b': nrm(ks[8], (L, GLA_KEY), 0.1),
        'gla_norm_g': 1.0 + nrm(ks[9], (L, GLA_VAL), 0.02),
        'gla_w_o': nrm(ks[10], (L, GLA_VAL, D), GLA_VAL ** -0.5),
        'rwkv_mu': jax.random.uniform(ks[11], (L, RWKV_COLS), f32, 0.0, 1.0),
        'rwkv_w0': jax.random.uniform(ks[12], (L, RWKV_DIM), f32, -5.0, 0.0),
        'rwkv_w2': nrm(ks[13], (L, RWKV_DECAY_RANK, RWKV_DIM), 0.5 * RWKV_DECAY_RANK ** -0.5),
        'rwkv_a0': nrm(ks[14], (L, RWKV_DIM), 0.1),
        'rwkv_a2': nrm(ks[15], (L, RWKV_A_RANK, RWKV_DIM), RWKV_A_RANK ** -0.5),
        'rwkv_g2': nrm(ks[16], (L, RWKV_G_RANK, RWKV_DIM), RWKV_G_RANK ** -0.5),
        'rwkv_k_k': 0.85 + nrm(ks[17], (L, RWKV_DIM), 0.02),
        'rwkv_k_a': 1.0 + nrm(ks[18], (L, RWKV_DIM), 0.02),
        'rwkv_r_k': nrm(ks[19], (L, RWKV_HEADS, RWKV_HEAD), 0.1),
        'rwkv_lnx_g': 1.0 + nrm(ks[20], (L, RWKV_DIM), 0.02),
        'rwkv_lnx_b': nrm(ks[21], (L, RWKV_DIM), 0.01),
        'rwkv_w_o': nrm(ks[22], (L, RWKV_DIM, D), RWKV_DIM ** -0.5),
        'w_out': nrm(ks[23], (L, D, D), D ** -0.5),
        'ffn_w1': nrm(ks[24], (N_DENSE, D, FFN_DIM), D ** -0.5),
        'ffn_w3': nrm(ks[25], (N_DENSE, D, FFN_DIM), D ** -0.5),
        'ffn_w2': nrm(ks[26], (N_DENSE, FFN_DIM, D), FFN_DIM ** -0.5),
        'moe_router': nrm(ks[27], (N_MOE, D, N_EXPERTS), D ** -0.5),
        'moe_w1': nrm(ks[28], (N_MOE, N_EXPERTS, D, MOE_FFN), D ** -0.5),
        'moe_w3': nrm(ks[29], (N_MOE, N_EXPERTS, D, MOE_FFN), D ** -0.5),
        'moe_w2': nrm(ks[30], (N_MOE, N_EXPERTS, MOE_FFN, D), MOE_FFN ** -0.5),
        'lnf_g': 1.0 + nrm(ks[31], (D,), 0.02),
    }


def reference(x, c, w_ada, b_ada, ln1_g, ln2_g, w_in, gla_alpha_up, gla_alpha_b, gla_norm_g, gla_w_o,
              rwkv_mu, rwkv_w0, rwkv_w2, rwkv_a0, rwkv_a2, rwkv_g2, rwkv_k_k, rwkv_k_a, rwkv_r_k,
              rwkv_lnx_g, rwkv_lnx_b, rwkv_w_o, w_out, ffn_w1, ffn_w3, ffn_w2,
              moe_router, moe_w1, moe_w3, moe_w2, lnf_g):
    c_act = jax.nn.silu(c)
    for l in range(DEPTH):
        mod = c_act @ w_ada[l] + b_ada[l]
        sh1, sc1, g1, sh2, sc2, g2 = [m[:, None, :] for m in _split(mod, (D_MODEL,) * 6)]
        h = _rmsnorm(x, ln1_g[l]) * (1.0 + sc1) + sh1
        x = x + g1 * _mixer(h, w_in[l], gla_alpha_up[l], gla_alpha_b[l], gla_norm_g[l], gla_w_o[l],
                            rwkv_mu[l], rwkv_w0[l], rwkv_w2[l], rwkv_a0[l], rwkv_a2[l], rwkv_g2[l],
                            rwkv_k_k[l], rwkv_k_a[l], rwkv_r_k[l], rwkv_lnx_g[l], rwkv_lnx_b[l],
                            rwkv_w_o[l], w_out[l])
        h = _rmsnorm(x, ln2_g[l]) * (1.0 + sc2) + sh2
        if l % 2 == 0:
            f = _swiglu(h, ffn_w1[l // 2], ffn_w3[l // 2], ffn_w2[l // 2])
        else:
            f = _moe(h, moe_router[l // 2], moe_w1[l // 2], moe_w3[l // 2], moe_w2[l // 2])
        x = x + g2 * f
    return _rmsnorm(x, lnf_g)
```

```python
import contextlib
import numpy as np
import concourse.bass as bass
import concourse.mybir as mybir
from concourse.bass_utils import run_bass_kernel_spmd

F32 = mybir.dt.float32
BF16 = mybir.dt.bfloat16
AF = mybir.ActivationFunctionType
ALU = mybir.AluOpType

ENGS = ("pe", "act", "dve", "pool", "sp")
D = 1024
N_IN = 6928
FFN_DIM = 2816
MOE_FFN = 3584
NEXP = 8
ROFF = 3088
GAOFF = 4880
GBOFF = 5904
EXPM05 = 0.6065306597126334


class Buf:
    __slots__ = ("w", "r")

    def __init__(self):
        self.w = None
        self.r = {}


class Prog:
    def __init__(self, nc, n_dma_sems=32, same_engine_sync=True):
        self.nc = nc
        self.streams = {e: [] for e in ENGS}
        self.cnt = {}
        self.seen = {e: {} for e in ENGS}
        self.semnames = list(ENGS[:4]) + ["dma%d" % i for i in range(n_dma_sems)]
        for k in self.semnames:
            self.cnt[k] = 0
        self.same_engine_sync = same_engine_sync
        self.n_dma = n_dma_sems
        self.dma_rr = 0
        self.n_instr = 0
        self.n_wait = 0
        self.pending = {e: False for e in ENGS}
        self.snap_n = {e: [] for e in ENGS[:4]}
        self.snap_d = {e: [] for e in ENGS[:4]}
        self.dirty = {e: True for e in ENGS}

    def _inherit(self, eng, key, val):
        ns = self.snap_n.get(key)
        if not ns:
            return
        import bisect
        i = bisect.bisect_right(ns, val) - 1
        if i < 0:
            return
        se = self.seen[eng]
        for k2, v2 in self.snap_d[key][i].items():
            if k2 != eng and se.get(k2, 0) < v2:
                se[k2] = v2

    def _need(self, eng, deps):
        for key, val in deps.items():
            if val <= 0:
                continue
            if key == eng and (eng == "pe" or not self.same_engine_sync):
                continue
            if self.seen[eng].get(key, 0) >= val:
                continue
            self.seen[eng][key] = val
            self.dirty[eng] = True
            self.streams[eng].append(("wait", key, val))
            self.n_wait += 1
            if key != eng:
                self._inherit(eng, key, val)

    @staticmethod
    def _collect(reads, writes):
        deps = {}
        for b in reads:
            if b.w is not None and deps.get(b.w[0], 0) < b.w[1]:
                deps[b.w[0]] = b.w[1]
        for b in writes:
            if b.w is not None and deps.get(b.w[0], 0) < b.w[1]:
                deps[b.w[0]] = b.w[1]
            for k, v in b.r.items():
                if deps.get(k, 0) < v:
                    deps[k] = v
        return deps

    def op(self, eng, fn, reads=(), writes=(), inc=True):
        self._need(eng, self._collect(reads, writes))
        if inc:
            self.cnt[eng] += 1
            v = self.cnt[eng]
            self.pending[eng] = False
            if self.dirty[eng]:
                self.snap_n[eng].append(v)
                self.snap_d[eng].append(dict(self.seen[eng]))
                self.dirty[eng] = False
        else:
            v = self.cnt[eng] + 1
            self.pending[eng] = True
        self.streams[eng].append(("op", fn, eng, 1 if inc else 0))
        self.n_instr += 1
        for b in reads:
            if b.r.get(eng, 0) < v:
                b.r[eng] = v
        for b in writes:
            b.w = (eng, v)
            b.r = {}

    def dma(self, fn, reads=(), writes=(), q="sp"):
        self._need(q, self._collect(reads, writes))
        semkey = "dma%d" % self.dma_rr
        self.dma_rr = (self.dma_rr + 1) % self.n_dma
        self._need(q, {semkey: self.cnt[semkey]})
        self.cnt[semkey] += 16
        v = self.cnt[semkey]
        self.streams[q].append(("op", fn, semkey, 16))
        self.n_instr += 1
        for b in reads:
            if b.r.get(semkey, 0) < v:
                b.r[semkey] = v
        for b in writes:
            b.w = (semkey, v)
            b.r = {}

    def barrier(self):
        assert not any(self.pending.values()), self.pending
        deps = dict(self.cnt)
        for e in ENGS:
            self._need(e, deps)

    def emit(self):
        nc = self.nc
        with contextlib.ExitStack() as st:
            sems = {k: st.enter_context(nc.semaphore("s_" + k)) for k in self.semnames}
            block = st.enter_context(nc.Block())

            def run(stream, engobj):
                for it in stream:
                    if it[0] == "wait":
                        engobj.wait_ge(sems[it[1]], it[2])
                    elif it[3]:
                        it[1](engobj).then_inc(sems[it[2]], it[3])
                    else:
                        it[1](engobj)

            block.tensor(lambda e: run(self.streams["pe"], e))
            block.scalar(lambda e: run(self.streams["act"], e))
            block.vector(lambda e: run(self.streams["dve"], e))
            block.gpsimd(lambda e: run(self.streams["pool"], e))
            block.sync(lambda e: run(self.streams["sp"], e))


class Ring:
    def __init__(self, tiles):
        self.tiles = tiles
        self.bufs = [Buf() for _ in tiles]
        self.i = 0

    def next(self):
        t, b = self.tiles[self.i], self.bufs[self.i]
        self.i = (self.i + 1) % len(self.tiles)
        return t, b


class KB:
    def __init__(self, S, L, neu_fp32=True, debug=False):
        self.debug = debug
        self.cstop = 9
        self.S, self.L = S, L
        self.NB = 2
        self.NT = 2 * S
        self.neu_dt = F32 if neu_fp32 else BF16
        self.nc = bass.Bass("TRN2", target_bir_lowering=False)
        import os as _os2
        self.P = Prog(self.nc, same_engine_sync=not _os2.environ.get("NOSES"))
        self.uid = 0
        self.rr = 0

    def name(self, s):
        self.uid += 1
        return "%s_%d" % (s, self.uid)

    def sb(self, st, shape, dt=F32, name="t"):
        return st.enter_context(self.nc.sbuf_tensor(self.name(name), list(shape), dt))

    def ps(self, st, shape, dt=F32, name="p"):
        return st.enter_context(self.nc.psum_tensor(self.name(name), list(shape), dt))

    def ring(self, st, shape, dt, n, name="r"):
        return Ring([self.sb(st, shape, dt, name) for _ in range(n)])

    def psring(self, st, shape, dt, n, name="pr"):
        return Ring([self.ps(st, shape, dt, name) for _ in range(n)])

    def mm(self, out, lhsT, rhs, start, stop, reads, writes):
        self.P.op("pe", lambda e: e.matmul(out, lhsT, rhs, start=start, stop=stop), reads, writes, inc=bool(stop))

    def tr(self, out, in_, ident, reads, writes):
        self.P.op("pe", lambda e: e.transpose(out, in_, ident), reads, writes)

    def act(self, out, in_, func, reads, writes, bias=None, scale=None):
        kw = {}
        if bias is not None:
            kw["bias"] = bias
        if scale is not None:
            kw["scale"] = scale
        self.P.op("act", lambda e: e.activation(out, in_, func, **kw), reads, writes)

    def tt(self, eng, out, a, b, op, reads, writes):
        self.P.op(eng, lambda e: e.tensor_tensor(out, a, b, op), reads, writes)

    def ts(self, eng, out, a, s1, s2, op0, op1, reads, writes):
        if s2 is None:
            self.P.op(eng, lambda e: e.tensor_scalar(out, a, s1, None, op0), reads, writes)
        else:
            self.P.op(eng, lambda e: e.tensor_scalar(out, a, s1, s2, op0, op1), reads, writes)

    def stt(self, eng, out, a, s, b, op0, op1, reads, writes):
        self.P.op(eng, lambda e: e.scalar_tensor_tensor(out, a, s, b, op0, op1), reads, writes)

    def cp(self, eng, out, in_, reads, writes):
        if eng == "act":
            self.P.op("act", lambda e: e.copy(out, in_), reads, writes)
        else:
            self.P.op(eng, lambda e: e.tensor_copy(out, in_), reads, writes)

    def dma(self, out, in_, reads=(), writes=(), slow=False, q="sp"):
        scr = (self.b_xT, self.b_pT, self.b_yaT)
        reads = [r for r in reads if r not in scr]
        writes = [w for w in writes if w not in scr]
        if slow:
            self.P.dma(lambda e: e.dma_start(out=out, in_=in_, allow_slow_non_contiguous=True), reads, writes, q=q)
        else:
            self.P.dma(lambda e: e.dma_start(out=out, in_=in_), reads, writes, q=q)

    def evac_eng(self):
        self.rr += 1
        return "act" if self.rr % 2 else "dve"

    def declare(self):
        nc, L, NT = self.nc, self.L, self.NT
        ND, NM = (L + 1) // 2, L // 2
        shapes = {
            "x": [NT, D], "c": [2, D], "w_ada": [L, D, 6 * D], "b_ada": [L, 6 * D], "ln1_g": [L, D], "ln2_g": [L, D],
            "w_in": [L, D, N_IN], "gla_alpha_up": [L, 16, 512], "gla_alpha_b": [L, 512], "gla_norm_g": [L, D],
            "gla_w_o": [L, D, D], "rwkv_mu": [L, 1792], "rwkv_w0": [L, 512], "rwkv_w2": [L, 64, 512],
            "rwkv_a0": [L, 512], "rwkv_a2": [L, 64, 512], "rwkv_g2": [L, 128, 512], "rwkv_k_k": [L, 512],
            "rwkv_k_a": [L, 512], "rwkv_r_k": [L, 512], "rwkv_lnx_g": [L, 512], "rwkv_lnx_b": [L, 512],
            "rwkv_w_o": [L, 512, D], "w_out": [L, D, D], "ffn_w1": [max(ND, 1), D, FFN_DIM], "ffn_w3": [max(ND, 1), D, FFN_DIM],
            "ffn_w2": [max(ND, 1), FFN_DIM, D], "moe_router": [max(NM, 1), D, NEXP],
            "moe_w1": [max(NM, 1) * NEXP, D, MOE_FFN], "moe_w3": [max(NM, 1) * NEXP, D, MOE_FFN],
            "moe_w2": [max(NM, 1) * NEXP, MOE_FFN, D], "lnf_g": [D],
            "k_ident": [128, 128], "k_masks": [128, 5, 512], "k_bones": [128, 2, 128], "k_sel": [8, 8, 128],
            "k_scanm": [128, 1024],
        }
        self.din = {k: nc.dram_tensor(k, v, F32, kind="ExternalInput").ap() for k, v in shapes.items()}
        self.in_shapes = shapes
        self.out = nc.dram_tensor("out", [NT, D], F32, kind="ExternalOutput").ap()
        kd = dict(kind="ExternalOutput") if self.debug else {}
        self.xT = nc.dram_tensor("xT_scr", [D, NT], F32, **kd).ap()
        self.pT = nc.dram_tensor("pT_scr", [N_IN, NT], F32, **kd).ap()
        self.yaT = nc.dram_tensor("yaT_scr", [D, NT], F32, **kd).ap()
        self.mod_scr = nc.dram_tensor("mod_scr", [L, 2, 6 * D], F32).ap()
        self.b_xT, self.b_pT, self.b_yaT, self.b_mod = Buf(), Buf(), Buf(), Buf()

    def setup_consts(self, st):
        P, din, L = self.P, self.din, self.L
        self.identf = self.sb(st, [128, 128], F32, "identf")
        self.identb = self.sb(st, [128, 128], BF16, "identb")
        self.masks = self.sb(st, [128, 5, 512], F32, "masks")
        self.bonesf = self.sb(st, [128, 2, 128], F32, "bonesf")
        self.bones = self.sb(st, [128, 2, 128], BF16, "bones")
        self.onesD = self.sb(st, [128, 128], BF16, "onesD")
        self.ones256 = self.sb(st, [128, 128], BF16, "ones256")
        self.sel = self.sb(st, [8, 8, 128], F32, "sel")
        self.scanm = self.sb(st, [128, 1024], F32, "scanm")
        self.cst = self.sb(st, [128, 8], F32, "cst")
        self.bc = Buf()
        cb = [self.bc]
        self.dma(self.identf[:], din["k_ident"], writes=cb)
        self.dma(self.masks[:], din["k_masks"], writes=cb)
        self.dma(self.bonesf[:], din["k_bones"], writes=cb)
        self.dma(self.sel[:], din["k_sel"], writes=cb)
        self.dma(self.scanm[:], din["k_scanm"], writes=cb)
        self.cp("dve", self.identb[:], self.identf[:], cb, cb)
        self.cp("dve", self.bones[:], self.bonesf[:], cb, cb)
        P.op("pool", lambda e: e.memset(self.onesD[:], 1.0 / 1024), (), cb)
        P.op("pool", lambda e: e.memset(self.ones256[:], 1.0 / 256), (), cb)
        for i, v in enumerate([1.0, 1e-6, 1e-5, 64e-5, 1e-24, 0.0]):
            P.op("pool", lambda e, i=i, v=v: e.memset(self.cst[:, i:i + 1], v), (), cb)
        self.vec = {}

        def colvec(key, n, nl=L):
            t = self.sb(st, [128, nl, n // 128], F32, "v_" + key)
            for l in range(nl):
                src = din[key][l] if nl > 1 or len(self.in_shapes[key]) == 2 else din[key]
                for c in range(n // 128):
                    self.dma(t[:, l, c:c + 1], src[c * 128:(c + 1) * 128].rearrange("(p o) -> p o", o=1), writes=[Buf()], slow=True, q="pool")
            self.vec[key] = t
        for key, n in [("ln1_g", D), ("ln2_g", D), ("gla_alpha_b", 512), ("gla_norm_g", D), ("rwkv_w0", 512),
                       ("rwkv_a0", 512), ("rwkv_k_k", 512), ("rwkv_k_a", 512), ("rwkv_r_k", 512),
                       ("rwkv_lnx_g", 512), ("rwkv_lnx_b", 512)]:
            colvec(key, n)
        t = self.sb(st, [128, 1, 8], F32, "v_lnf")
        for c in range(8):
            self.dma(t[:, 0, c:c + 1], din["lnf_g"][c * 128:(c + 1) * 128].rearrange("(p o) -> p o", o=1), writes=[Buf()], slow=True, q="pool")
        self.vec["lnf_g"] = t
        pieces = [(0, 128), (128, 128), (256, 128), (384, 128), (512, 64), (576, 128), (704, 128), (832, 128), (960, 128),
                  (1088, 128), (1216, 128), (1344, 128), (1472, 128), (1600, 64), (1664, 128)]
        self.mu_piece = {}
        mu = self.sb(st, [128, L, 15], F32, "v_mu2")
        for l in range(L):
            for i, (o, n) in enumerate(pieces):
                self.dma(mu[0:n, l, i:i + 1], din["rwkv_mu"][l][o:o + n].rearrange("(p o) -> p o", o=1), writes=[Buf()], slow=True, q="pool")
        self.vec["mu"] = mu
        nab = self.sb(st, [128, L, 4], F32, "v_nab")
        self.vec["neg_alpha_b"] = nab
        self.modT = self.sb(st, [128, L, 48, 2], F32, "modT")
        self.sc1 = self.sb(st, [128, L, 8, 2], F32, "sc1")
        self.sc2 = self.sb(st, [128, L, 8, 2], F32, "sc2")

    def prologue(self):
        P, din, L, NT = self.P, self.din, self.L, self.NT
        cb = [self.bc]
        with contextlib.ExitStack() as st:
            cT = self.sb(st, [128, 8, 2], F32, "cT")
            bcT = Buf()
            for b in range(2):
                for k in range(8):
                    self.dma(cT[:, k, b:b + 1], din["c"][b, k * 128:(k + 1) * 128].rearrange("(p o) -> p o", o=1), writes=[bcT], slow=True)
            self.act(cT[:], cT[:], AF.Silu, [bcT], [bcT])
            wst = self.ring(st, [128, 8, 512], F32, 4, "wada")
            psr = self.psring(st, [128, 512], F32, 2, "psada")
            modsb = self.sb(st, [2, 6 * D], F32, "modsb")
            bada = self.sb(st, [2, 6 * D], F32, "bada")
            bmod, bbada = Buf(), Buf()
            for l in range(L):
                self.dma(bada[:], din["b_ada"][l:l + 1, :].partition_broadcast(2), writes=[bbada])
                for g in range(12):
                    w, bw = wst.next()
                    self.dma(w[:], din["w_ada"][l][:, g * 512:(g + 1) * 512].rearrange("(k p) n -> p k n", p=128), writes=[bw],
                             q="sp" if g % 2 == 0 else "act")
                    ps, bps = psr.next()
                    for k in range(8):
                        self.mm(ps[0:2, :], cT[:, k, :], w[:, k, :], k == 0, k == 7, [bcT, bw], [bps])
                    self.tt("dve", modsb[:, g * 512:(g + 1) * 512], ps[0:2, :], bada[:, g * 512:(g + 1) * 512], ALU.add,
                            [bps, bbada], [bmod])
                bml = Buf()
                self.dma(self.mod_scr[l], modsb[:], reads=[bmod], writes=[bml], q="act")
                for b in range(2):
                    for cch in range(48):
                        self.dma(self.modT[:, l, cch, b:b + 1],
                                 self.mod_scr[l, b, cch * 128:(cch + 1) * 128].rearrange("(p o) -> p o", o=1),
                                 reads=[bml], writes=[Buf()], slow=True, q="pool")
            P.barrier()
            self.ts("dve", self.vec["neg_alpha_b"][:], self.vec["gla_alpha_b"][:], -1.0, None, ALU.mult, None, cb, cb)
            for l in range(L):
                for b in range(2):
                    self.stt("dve", self.sc1[:, l, :, b], self.modT[:, l, 8:16, b], 1.0, self.vec["ln1_g"][:, l, :], ALU.add, ALU.mult, cb, cb)
                    self.stt("dve", self.sc2[:, l, :, b], self.modT[:, l, 32:40, b], 1.0, self.vec["ln2_g"][:, l, :], ALU.add, ALU.mult, cb, cb)
        P.barrier()
        with contextlib.ExitStack() as st:
            xin = self.ring(st, [128, D], F32, 3, "xin")
            pst = self.psring(st, [128, 512], F32, 4, "pst")
            xo = self.ring(st, [128, 8, 512], F32, 2, "xo")
            for t0 in range(0, NT, 512):
                o, bo = xo.next()
                for j in range(4):
                    xi, bxi = xin.next()
                    self.dma(xi[:], din["x"][t0 + j * 128: t0 + (j + 1) * 128, :], writes=[bxi])
                    for half in range(2):
                        ps, bps = pst.next()
                        for q in range(4):
                            cch = half * 4 + q
                            self.tr(ps[:, q * 128:(q + 1) * 128], xi[:, cch * 128:(cch + 1) * 128], self.identf[:], [bxi, self.bc], [bps])
                        self.cp(self.evac_eng(), o[:, half * 4:(half + 1) * 4, j * 128:(j + 1) * 128],
                                ps[:].rearrange("p (q t) -> p q t", q=4), [bps], [bo])
                self.dma(self.xT[:, t0:t0 + 512].rearrange("(c p) t -> p c t", p=128), o[:], reads=[bo], writes=[self.b_xT], q="act")
        P.barrier()

    def norm_tile(self, st_objs, l, which, t0, TA, b, h_out, bh, h32_out=None, bh32=None):
        xt, bx, sq, bsq, psn, bpsn, rstd, brs, tmp, btmp = st_objs
        scale = (self.sc1 if which == 1 else self.sc2)
        shift_off = 0 if which == 1 else 24
        cb = self.bc
        self.dma(xt[:, :, 0:TA], self.xT[:, t0:t0 + TA].rearrange("(c p) t -> p c t", p=128), reads=[self.b_xT], writes=[bx])
        self.act(sq[:, :, 0:TA], xt[:, :, 0:TA], AF.Square, [bx], [bsq])
        for k in range(8):
            self.mm(psn[:, 0:TA], self.onesD[:], sq[:, k, 0:TA], k == 0, k == 7, [bsq, cb], [bpsn])
        self.act(rstd[:, 0:TA], psn[:, 0:TA], AF.Ln, [bpsn, cb], [brs], bias=self.cst[:, 1:2])
        self.act(rstd[:, 0:TA], rstd[:, 0:TA], AF.Exp, [brs], [brs], scale=-0.5)
        for k in range(8):
            eng = "dve" if k % 2 == 0 else "pool"
            self.tt(eng, tmp[:, k, 0:TA], xt[:, k, 0:TA], rstd[:, 0:TA], ALU.mult, [bx, brs], [btmp])
        for k in range(8):
            if h32_out is not None:
                self.act(h32_out[:, k, 0:TA], tmp[:, k, 0:TA], AF.Identity, [btmp, cb], [bh32],
                         bias=self.modT[:, l, shift_off + k, b:b + 1], scale=scale[:, l, k, b:b + 1])
                self.cp("pool", h_out[:, k, :], h32_out[:, k, 0:TA], [bh32], [bh])
            else:
                self.act(h_out[:, k, :], tmp[:, k, 0:TA], AF.Identity, [btmp, cb], [bh],
                         bias=self.modT[:, l, shift_off + k, b:b + 1], scale=scale[:, l, k, b:b + 1])

    def norm_objs(self, st, TA):
        xt = self.sb(st, [128, 8, TA], F32, "nx")
        sq = self.sb(st, [128, 8, TA], BF16, "nsq")
        psn = self.ps(st, [128, 512], F32, "npsn")
        rstd = self.sb(st, [128, TA], F32, "nrstd")
        tmp = self.sb(st, [128, 8, TA], F32, "ntmp")
        return (xt, Buf(), sq, Buf(), psn, Buf(), rstd, Buf(), tmp, Buf())

    def phase_A(self, l):
        P, din, NT, S = self.P, self.din, self.NT, self.S
        TA = min(512, S)
        with contextlib.ExitStack() as st:
            hT = self.sb(st, [128, 8, NT], BF16, "hT")
            bhs = [Buf() for _ in range(NT // TA)]
            objs = self.norm_objs(st, TA)
            wst = self.ring(st, [128, 8, 512], F32, 2, "wst")
            wbf = self.ring(st, [128, 8, 512], BF16, 2, "wbf")
            psr = self.psring(st, [128, 512], F32, 4, "psA")
            ost = self.ring(st, [128, 512], F32, 4, "ost")

            def load_chunk(c0):
                ncol = min(512, N_IN - c0)
                w, bw = wst.next()
                self.dma(w[:, :, 0:ncol], din["w_in"][l][:, c0:c0 + ncol].rearrange("(k p) n -> p k n", p=128), writes=[bw])
                wb, bwb = wbf.next()
                for k in range(8):
                    self.cp("pool" if k % 2 else "act", wb[:, k, 0:ncol], w[:, k, 0:ncol], [bw], [bwb])
                return wb, bwb, ncol

            def compute(c0, wb, bwb, ncol, t0):
                for cc in range(0, ncol, 128):
                    m = min(128, ncol - cc)
                    ps, bps = psr.next()
                    for k in range(8):
                        self.mm(ps[0:m, :], wb[:, k, cc:cc + m], hT[:, k, t0:t0 + 512], k == 0, k == 7,
                                [bwb] + bhs[t0 // TA:(t0 + 512) // TA], [bps])
                    o, bo = ost.next()
                    self.cp(self.evac_eng(), o[0:m, :], ps[0:m, :], [bps], [bo])
                    self.dma(self.pT[c0 + cc:c0 + cc + m, t0:t0 + 512], o[0:m, :], reads=[bo], writes=[self.b_pT], q="act")

            first = load_chunk(0)
            for t0 in range(0, NT, TA):
                self.norm_tile(objs, l, 1, t0, TA, t0 // S, hT[:, :, t0:t0 + TA], bhs[t0 // TA])
                if (t0 + TA) % 512 == 0:
                    compute(0, *first, t0 + TA - 512)
            for c0 in range(512, N_IN, 512):
                wb, bwb, ncol = load_chunk(c0)
                for t0 in range(0, NT, 512):
                    compute(c0, wb, bwb, ncol, t0)
        P.barrier()

    def phase_B(self, l):
        P, din, NT, S = self.P, self.din, self.NT, self.S
        cb = self.bc
        ST = min(256, S)
        NTL = ST // 128
        with contextlib.ExitStack() as st:
            wo = self.sb(st, [128, 8, D], BF16, "gwo")
            bwo = Buf()
            with contextlib.ExitStack() as st2:
                wst = self.ring(st2, [128, 2, D], F32, 2, "gwst")
                for k2 in range(4):
                    w, bw = wst.next()
                    self.dma(w[:], din["gla_w_o"][l][k2 * 256:(k2 + 1) * 256, :].rearrange("(k p) n -> p k n", p=128), writes=[bw])
                    for kk in range(2):
                        k = k2 * 2 + kk
                        self.act(wo[:, k, :], w[:, kk, :], AF.Identity, [bw, cb], [bwo], scale=self.vec["gla_norm_g"][:, l, k:k + 1])
                P.barrier()
            aup = self.sb(st, [16, 512], F32, "aup")
            baup = Buf()
            self.dma(aup[:], din["gla_alpha_up"][l], writes=[baup])
            ps_a = self.psring(st, [128, 512], F32, 2, "gpsa")
            ps_tk = self.ps(st, [128, 1024], BF16, "gpstk"); bptk = Buf()
            ps_tv, bptv = ps_tk, bptk
            ps_o2 = [self.ps(st, [128, 1024], F32, "gpso") for _ in range(2)]
            ps_s = self.psring(st, [128, 512], F32, 1, "gpss")
            pT = self.pT

            def seq_gen(b):
                ps_o = ps_o2[b]; bpo = Buf()
                S32 = self.sb(st, [128, 4, 256], F32, "S32")
                Sbf = self.sb(st, [128, 4, 256], BF16, "Sbf")
                bS = [Buf() for _ in range(4)]
                stmp = self.ring(st, [128, 256], F32, 2, "stmp")
                q = self.sb(st, [128, 4, ST], F32, "qT"); bq = Buf()
                k_ = self.sb(st, [128, 4, ST], F32, "kT"); bk = Buf()
                v = self.sb(st, [128, 8, ST], F32, "vT"); bv = Buf()
                g = self.sb(st, [128, 8, ST], F32, "gT"); bg = Buf()
                pa = self.sb(st, [16, ST], F32, "paT"); bpa = Buf()
                gar = self.ring(st, [128, ST], F32, 2, "gar")
                lT = self.sb(st, [128, 4, ST], F32, "lT"); blT = Buf()
                cs = self.sb(st, [128, 4, ST], F32, "cs"); bcs = Buf()
                ebT = self.sb(st, [128, 4, ST], F32, "ebT"); beb = Buf()
                enbT = self.sb(st, [128, 4, ST], F32, "enbT"); benb = Buf()
                qeT = self.sb(st, [128, 4, ST], BF16, "qeT"); bqe = Buf()
                keT = self.sb(st, [128, 4, ST], BF16, "keT"); bke = Buf()
                vTb = self.sb(st, [128, 8, ST], BF16, "vTb"); bvb = Buf()
                ofin = self.sb(st, [128, 8, ST], BF16, "ofin"); bof = Buf()
                ketok = self.ring(st, [128, 512], BF16, 2, "ketok")
                vtok = self.ring(st, [128, 1024], BF16, 1, "vtok")
                attm = self.ring(st, [128, 512], BF16, 2, "attm")
                sq = self.ring(st, [128, 1024], BF16, 1, "gsq")
                rstd = self.ring(st, [128, 512], F32, 1, "grstd")
                of = self.ring(st, [128, 1024], F32, 1, "gof")
                ost = self.ring(st, [128, ST], F32, 2, "gost")
                for h in range(4):
                    P.op("pool", lambda e, h=h: e.memset(S32[:, h, :], 0.0), (), [bS[h]])
                    P.op("pool", lambda e, h=h: e.memset(Sbf[:, h, :], 0.0), (), [bS[h]])
                for s0 in range(0, S, ST):
                    t0 = b * S + s0
                    self.dma(q[:], pT[0:512, t0:t0 + ST].rearrange("(h p) t -> p h t", p=128), reads=[self.b_pT], writes=[bq])
                    self.dma(k_[:], pT[512:1024, t0:t0 + ST].rearrange("(h p) t -> p h t", p=128), reads=[self.b_pT], writes=[bk])
                    self.dma(v[:], pT[1024:2048, t0:t0 + ST].rearrange("(h p) t -> p h t", p=128), reads=[self.b_pT], writes=[bv])
                    self.dma(g[:], pT[2048:3072, t0:t0 + ST].rearrange("(h p) t -> p h t", p=128), reads=[self.b_pT], writes=[bg])
                    self.dma(pa[:], pT[3072:3088, t0:t0 + ST], reads=[self.b_pT], writes=[bpa])
                    yield
                    for h in range(4):
                        ps, bps = ps_a.next()
                        self.mm(ps[:, 0:ST], aup[:, h * 128:(h + 1) * 128], pa[:, :], True, True, [baup, bpa], [bps])
                        self.act(lT[:, h, :], ps[:, 0:ST], AF.Exp, [bps, cb], [blT], bias=self.vec["neg_alpha_b"][:, l, h:h + 1], scale=-1.0)
                        if h % 2:
                            yield
                    self.act(lT[:], lT[:], AF.Ln, [blT, cb], [blT], bias=self.cst[:, 0:1])
                    yield
                    P.op("dve", lambda e: e.tensor_tensor_scan(cs[:].rearrange("p h t -> p (h t)"), self.scanm[:, 0:4 * ST],
                                                               lT[:].rearrange("p h t -> p (h t)"), 0.0, ALU.mult, ALU.add),
                         [blT, cb], [bcs])
                    yield
                    self.act(ebT[:], cs[:], AF.Exp, [bcs], [beb], scale=-1.0 / 16)
                    self.act(enbT[:], cs[:], AF.Exp, [bcs], [benb], scale=1.0 / 16)
                    self.cp("act", vTb[:], v[:], [bv], [bvb])
                    yield
                    self.stt("dve", qeT[:], q[:], 128 ** -0.5, ebT[:], ALU.mult, ALU.mult, [bq, beb], [bqe])
                    self.tt("dve", keT[:], k_[:], enbT[:], ALU.mult, [bk, benb], [bke])
                    self.act(g[:], g[:], AF.Silu, [bg], [bg])
                    sg, bsg = g, bg
                    yield
                    for j in range(NTL):
                        c0 = j * 128
                        kt, bkt = ketok.next()
                        vt, bvt = vtok.next()
                        for h in range(4):
                            self.tr(ps_tk[:, h * 128:(h + 1) * 128], keT[:, h, c0:c0 + 128], self.identb[:], [bke, cb], [bptk])
                        self.cp("act", kt[:], ps_tk[:, 0:512], [bptk], [bkt])
                        for c in range(8):
                            self.tr(ps_tv[:, c * 128:(c + 1) * 128], vTb[:, c, c0:c0 + 128], self.identb[:], [bvb, cb], [bptv])
                        self.cp("dve", vt[:], ps_tv[:], [bptv], [bvt])
                        ps, bps = ps_a.next()
                        for h in range(4):
                            self.mm(ps[:, h * 128:(h + 1) * 128], keT[:, h, c0:c0 + 128], qeT[:, h, c0:c0 + 128], True, True, [bke, bqe], [bps])
                        am, bam = attm.next()
                        self.tt("dve", am[:], ps[:], self.masks[:, 0, :], ALU.mult, [bps, cb], [bam])
                        yield
                        for ch in range(2):
                            r0 = ch * 64
                            for h in range(4):
                                for vc in range(2):
                                    o_ap = ps_o[:, (h * 2 + vc) * 128 + r0:(h * 2 + vc) * 128 + r0 + 64]
                                    self.mm(o_ap, Sbf[:, h, vc * 128:(vc + 1) * 128], qeT[:, h, c0 + r0:c0 + r0 + 64], True, False,
                                            [bS[h], bqe], [bpo])
                                    self.mm(o_ap, vt[r0:r0 + 64, h * 256 + vc * 128:h * 256 + (vc + 1) * 128],
                                            am[r0:r0 + 64, h * 128 + r0:h * 128 + r0 + 64], False, True, [bvt, bam], [bpo])
                            for h in range(4):
                                pss, bpss = ps_s.next()
                                self.mm(pss[:, 0:256], kt[r0:r0 + 64, h * 128:(h + 1) * 128], vt[r0:r0 + 64, h * 256:(h + 1) * 256], True, True,
                                        [bkt, bvt], [bpss])
                                tmp, btmp = stmp.next()
                                self.tt("dve", tmp[:], S32[:, h, :], pss[:, 0:256], ALU.add, [bS[h], bpss], [btmp])
                                dl = ebT[:, h, c0 + r0 + 63:c0 + r0 + 64]
                                self.ts("dve", S32[:, h, :], tmp[:], dl, None, ALU.mult, None, [btmp, beb], [bS[h]])
                                self.act(Sbf[:, h, :], tmp[:], AF.Identity, [btmp, beb], [bS[h]], scale=dl)
                                if h % 2:
                                    yield
                        sqt, bsq = sq.next()
                        self.act(sqt[:], ps_o[:], AF.Square, [bpo], [bsq])
                        yield
                        ps, bps = ps_a.next()
                        for h in range(4):
                            for vc in range(2):
                                self.mm(ps[:, h * 128:(h + 1) * 128], self.ones256[:], sqt[:, (h * 2 + vc) * 128:(h * 2 + vc + 1) * 128],
                                        vc == 0, vc == 1, [bsq, cb], [bps])
                        rs, brs = rstd.next()
                        self.act(rs[:], ps[:], AF.Ln, [bps, cb], [brs], bias=self.cst[:, 2:3])
                        yield
                        self.act(rs[:], rs[:], AF.Exp, [brs], [brs], scale=-0.5)
                        yield
                        oft, boft = of.next()
                        for vc in range(2):
                            self.tt("dve", oft[:].rearrange("p (h v t) -> p h v t", h=4, v=2)[:, :, vc, :],
                                    ps_o[:].rearrange("p (h v t) -> p h v t", h=4, v=2)[:, :, vc, :],
                                    rs[:].rearrange("p (h t) -> p h t", h=4), ALU.mult, [bpo, brs], [boft])
                        yield
                        self.tt("pool", ofin[:, :, c0:c0 + 128], oft[:].rearrange("p (c t) -> p c t", c=8), sg[:, :, c0:c0 + 128], ALU.mult,
                                [boft, bsg], [bof])
                        yield
                    for dc in range(8):
                        ga, bga = gar.next()
                        self.dma(ga[:], pT[GAOFF + dc * 128:GAOFF + (dc + 1) * 128, t0:t0 + ST], reads=[self.b_pT], writes=[bga])
                        self.act(ga[:], ga[:], AF.Sigmoid, [bga], [bga])
                        ps, bps = ps_a.next()
                        for k in range(8):
                            self.mm(ps[:, 0:ST], wo[:, k, dc * 128:(dc + 1) * 128], ofin[:, k, :], k == 0, k == 7, [bwo, bof], [bps])
                        o, bo = ost.next()
                        self.tt("dve", o[:, 0:ST], ps[:, 0:ST], ga[:], ALU.mult, [bps, bga], [bo])
                        self.dma(self.yaT[dc * 128:(dc + 1) * 128, t0:t0 + ST], o[:, 0:ST], reads=[bo], writes=[self.b_yaT], q="act")
                        if dc % 2:
                            yield

            active = [seq_gen(0), seq_gen(1)]
            while active:
                for gen in list(active):
                    try:
                        next(gen)
                    except StopIteration:
                        active.remove(gen)
        P.barrier()

    def phase_C(self, l):
        P, din, NT, S = self.P, self.din, self.NT, self.S
        cb = self.bc
        ST = min(256, S)
        NTL = ST // 128
        NDT = self.neu_dt
        V = self.vec
        with contextlib.ExitStack() as st:
            Xsb = self.sb(st, [128, 512], BF16, "Xsb"); Usb = self.sb(st, [128, 512], BF16, "Usb"); bX, bU = Buf(), Buf()
            P.op("pool", lambda e: e.memset(Xsb[:], 0.0), (), [bX])
            P.op("pool", lambda e: e.memset(Usb[:], 0.0), (), [bU])
            rwo = self.sb(st, [128, 4, D], BF16, "rwo"); wout = self.sb(st, [128, 8, D], BF16, "wout"); bw = Buf()
            w2 = self.sb(st, [64, 512], F32, "w2"); a2 = self.sb(st, [64, 512], F32, "a2")
            g2f = self.sb(st, [128, 512], F32, "g2f"); g2 = self.sb(st, [128, 512], BF16, "g2")
            with contextlib.ExitStack() as st2:
                wst = self.ring(st2, [128, 2, D], F32, 2, "cwst")
                for k2 in range(2):
                    w, bws = wst.next()
                    self.dma(w[:], din["rwkv_w_o"][l][k2 * 256:(k2 + 1) * 256, :].rearrange("(k p) n -> p k n", p=128), writes=[bws])
                    self.cp("pool", rwo[:, k2 * 2:k2 * 2 + 2, :], w[:], [bws], [bw])
                for k2 in range(4):
                    w, bws = wst.next()
                    self.dma(w[:], din["w_out"][l][k2 * 256:(k2 + 1) * 256, :].rearrange("(k p) n -> p k n", p=128), writes=[bws])
                    self.cp("pool", wout[:, k2 * 2:k2 * 2 + 2, :], w[:], [bws], [bw])
                self.dma(w2[:], din["rwkv_w2"][l], writes=[bw])
                self.dma(a2[:], din["rwkv_a2"][l], writes=[bw])
                self.dma(g2f[:], din["rwkv_g2"][l], writes=[bw])
                self.cp("pool", g2[:], g2f[:], [bw], [bw])
                P.barrier()
            H32 = [self.sb(st, [128, 4, 64], F32, "H32") for _ in range(2)]
            Hbf = [self.sb(st, [128, 4, 64], BF16, "Hbf") for _ in range(2)]
            Hbd = [self.sb(st, [128, 4, 128], BF16, "Hbd") for _ in range(2)]
            bH = [Buf(), Buf()]
            htmp = self.sb(st, [128, 4, 64], F32, "htmp"); bht = Buf()
            big = lambda nm, dt=F32: (self.sb(st, [128, 4, ST], dt, nm), Buf())
            W0 = self.sb(st, [128, 4, ST + 1], F32, "W0"); bW0 = Buf()
            W1 = self.sb(st, [128, 4, ST + 1], F32, "W1"); bW1 = Buf()
            W2 = self.sb(st, [128, 4, ST + 1], F32, "W2"); bW2 = Buf()
            rP, kP, vP = W0, W1, W2
            wdP = self.sb(st, [64, ST + 1], F32, "wdP"); adP = self.sb(st, [64, ST + 1], F32, "adP"); gdP = self.sb(st, [128, ST + 1], F32, "gdP")
            bsmP = Buf()
            T0, bT0 = big("T0")
            dif, bdif = T0, bT0
            tA, btA = T0, bT0
            rn, brn = T0, bT0
            r_, br = big("r"); k_, bk = big("k"); v_, bv = big("v")
            wd = self.sb(st, [64, ST], F32, "wd"); ad = self.sb(st, [64, ST], F32, "ad"); gd = self.sb(st, [128, ST], F32, "gd")
            gdb = self.sb(st, [128, ST], BF16, "gdb")
            bsm = Buf()
            sw, bsw = big("sw"); cs, bcs = big("cs"); a_, ba = big("a"); gg, bgg = big("gg")
            E1, bE1 = W0[:, :, 0:ST], bW0
            E2, bE2 = W1[:, :, 0:ST], bW1
            kkn, bkkn = W2[:, :, 0:ST], bW2
            E3, bE3 = big("E3")
            k2_, bk2 = big("k2")
            sqk, bsqk = big("sqk", BF16)
            At, bAt = big("At", BF16); Bt, bBt = big("Bt", BF16); Kt, bKt = big("Kt", BF16); Rt, bRt = big("Rt", BF16)
            vb, bvb = big("vb", BF16)
            bonus, bbon = big("bonus")
            yT, byT = big("yT")
            Vtok = self.ring(st, [128, 512], BF16, 2, "Vtok"); Btok = self.ring(st, [128, 512], BF16, 2, "Btok")
            Ktok = self.ring(st, [128, 512], BF16, 2, "Ktok")
            Vpad = self.sb(st, [128, 8, 128], BF16, "Vpad"); Upad = self.sb(st, [128, 8, 128], BF16, "Upad"); bVp = Buf(); bUp = Buf()
            P.op("pool", lambda e: e.memset(Vpad[:], 0.0), (), [bVp])
            P.op("pool", lambda e: e.memset(Upad[:], 0.0), (), [bUp])
            Nm = self.sb(st, [128, 8, 128], NDT, "Nm"); NTm = self.sb(st, [128, 8, 128], NDT, "NTm")
            Mm = [self.sb(st, [128, 8, 128], NDT, "Mm") for _ in range(2)]
            MTm = [self.sb(st, [128, 8, 128], NDT, "MTm") for _ in range(2)]
            Sm = [self.sb(st, [128, 8, 128], NDT, "Sm") for _ in range(2)] if NDT != F32 else [None, None]
            Sm32 = self.sb(st, [128, 8, 128], F32, "Sm32")
            bN, bNT, bM, bMT, bSm = Buf(), Buf(), [Buf(), Buf()], [Buf(), Buf()], [Buf(), Buf()]
            dbl = lambda nm: ([self.sb(st, [128, 8, 128], BF16, nm) for _ in range(2)], [Buf(), Buf()])
            Zb, bZ = dbl("Zb"); LakT, bLak = dbl("LakT"); LrbT, bLrb = dbl("LrbT"); LrkT, bLrk = dbl("LrkT")

            ybf, bybf = sqk, bsqk
            dd, bdd = sw, bsw
            sq2, bsq2 = vb, bvb
            rs2, brs2 = cs, bcs
            yfin, byfin = At, bAt
            gbr = self.ring(st, [128, ST], F32, 2, "gbr")
            yagr = self.ring(st, [128, ST], F32, 2, "yagr")
            xtr = self.ring(st, [128, ST], F32, 2, "xtr")
            mtmp = self.ring(st, [128, ST], F32, 2, "mtmp")
            merged = self.sb(st, [128, 8, ST], BF16, "merged"); bmg = Buf()
            pA = self.psring(st, [128, 512], F32, 2, "cpA")
            pY = self.ps(st, [128, 512], F32, "cpY"); bpY = Buf()
            pTb = self.ps(st, [128, 1024], BF16, "cpTb"); bpTb = Buf()
            pW = [self.ps(st, [128, 1024], F32, "cpW") for _ in range(2)]
            bpW = [Buf(), Buf()]
            pT_ = self.pT
            if self.debug:
                print("phase C sbuf remaining", self.nc.sbuf_bytes_remaining)

            def wide(i):
                return pW[i], bpW[i]

            for b in range(2):
                P.op("pool", lambda e: e.memset(H32[0][:], 0.0), (), [bH[0]])
                P.op("pool", lambda e: e.memset(Hbf[0][:], 0.0), (), [bH[0]])
                P.op("pool", lambda e: e.memset(Hbd[0][:], 0.0), (), [bH[0]])
                P.op("pool", lambda e: e.memset(Hbd[1][:], 0.0), (), [bH[1]])
                hp = 0
                for s0 in range(0, S, ST):
                    t0 = b * S + s0
                    R0 = ROFF
                    lo = 1 if s0 == 0 else 0

                    def ld(dst3, bdst, rows, r0, nchunk):
                        src = pT_[R0 + r0:R0 + r0 + rows, t0 - 1 + lo:t0 + ST]
                        if nchunk > 1:
                            if lo:
                                P.op("pool", lambda e: e.memset(dst3[:, :, 0:1], 0.0), (), [bdst])
                            self.dma(dst3[:, :, lo:ST + 1], src.rearrange("(c p) t -> p c t", p=128), reads=[self.b_pT], writes=[bdst])
                        else:
                            if lo:
                                P.op("pool", lambda e: e.memset(dst3[0:rows, 0:1], 0.0), (), [bdst])
                            self.dma(dst3[0:rows, lo:ST + 1], src, reads=[self.b_pT], writes=[bdst])
                    ld(rP, bW0, 512, 0, 4); ld(wdP, bsmP, 64, 512, 1); ld(kP, bW1, 512, 576, 4); ld(vP, bW2, 512, 1088, 4)
                    ld(adP, bsmP, 64, 1600, 1); ld(gdP, bsmP, 128, 1664, 1)
                    mu = V["mu"]
                    for src, bsrc, dst, bdst, mi in ((rP, bW0, r_, br, 0), (kP, bW1, k_, bk, 5), (vP, bW2, v_, bv, 9)):
                        self.tt("dve", dif[:], src[:, :, 0:ST], src[:, :, 1:ST + 1], ALU.subtract, [bsrc], [bdif])
                        for c in range(4):
                            self.stt("dve", dst[:, c, :], dif[:, c, :], mu[:, l, mi + c:mi + c + 1], src[:, c, 1:ST + 1], ALU.mult, ALU.add,
                                     [bdif, bsrc, cb], [bdst])
                    for src, dst, rows, mi in ((wdP, wd, 64, 4), (adP, ad, 64, 13), (gdP, gd, 128, 14)):
                        self.tt("dve", dif[0:rows, 0, :], src[0:rows, 0:ST], src[0:rows, 1:ST + 1], ALU.subtract, [bsmP], [bdif])
                        self.stt("dve", dst[0:rows, :], dif[0:rows, 0, :], mu[0:rows, l, mi:mi + 1], src[0:rows, 1:ST + 1], ALU.mult, ALU.add,
                                 [bdif, bsmP, cb], [bsm])
                    self.act(wd[:], wd[:], AF.Tanh, [bsm], [bsm])
                    for c in range(4):
                        ps, bps = pA.next()
                        self.mm(ps[:, 0:ST], w2[:, c * 128:(c + 1) * 128], wd[:, :], True, True, [bw, bsm], [bps])
                        self.act(sw[:, c, :], ps[:, 0:ST], AF.Sigmoid, [bps, cb], [bsw], bias=V["rwkv_w0"][:, l, c:c + 1])
                    for c in range(4):
                        ps, bps = pA.next()
                        self.mm(ps[:, 0:ST], a2[:, c * 128:(c + 1) * 128], ad[:, :], True, True, [bw, bsm], [bps])
                        self.act(a_[:, c, :], ps[:, 0:ST], AF.Sigmoid, [bps, cb], [ba], bias=V["rwkv_a0"][:, l, c:c + 1])
                    self.act(gdb[:], gd[:], AF.Sigmoid, [bsm], [bsm])
                    for c in range(4):
                        ps, bps = pA.next()
                        self.mm(ps[:, 0:ST], g2[:, c * 128:(c + 1) * 128], gdb[:, :], True, True, [bw, bsm], [bps])
                        self.cp("dve", gg[:, c, :], ps[:, 0:ST], [bps], [bgg])
                    P.op("dve", lambda e: e.tensor_tensor_scan(cs[:].rearrange("p h t -> p (h t)"), self.scanm[:, 0:4 * ST],
                                                               sw[:].rearrange("p h t -> p (h t)"), 0.0, ALU.mult, ALU.add),
                         [bsw, cb], [bcs])
                    self.tt("dve", tA[:], cs[:], sw[:], ALU.subtract, [bcs, bsw], [btA])
                    self.act(E1[:], tA[:], AF.Exp, [btA], [bE1], scale=-EXPM05)
                    self.act(E2[:], cs[:], AF.Exp, [bcs], [bE2], scale=EXPM05)
                    self.act(E3[:], cs[:], AF.Exp, [bcs], [bE3], scale=-EXPM05)
                    for c in range(4):
                        self.act(sqk[:, c, :], k_[:, c, :], AF.Square, [bk, cb], [bsqk], scale=V["rwkv_k_k"][:, l, c:c + 1])
                    for c in range(4):
                        ps, bps = pA.next()
                        self.mm(ps[:, 0:ST], self.bones[:, 0, :], sqk[:, c, :], True, True, [bsqk, cb], [bps])
                        self.act(rn[:, c, :], ps[:, 0:ST], AF.Ln, [bps, cb], [brn], bias=self.cst[:, 4:5])
                    self.act(rn[:], rn[:], AF.Exp, [brn], [brn], scale=-0.5)
                    for c in range(4):
                        self.stt("dve", kkn[:, c, :], k_[:, c, :], V["rwkv_k_k"][:, l, c:c + 1], rn[:, c, :], ALU.mult, ALU.mult,
                                 [bk, brn, cb], [bkkn])
                    for c in range(4):
                        self.ts("dve", tA[:, c, :], a_[:, c, :], -1.0, V["rwkv_k_a"][:, l, c:c + 1], ALU.add, ALU.mult, [ba, cb, btA], [btA])
                    self.stt("dve", k2_[:], tA[:], 1.0, k_[:], ALU.add, ALU.mult, [btA, bk], [bk2])
                    self.stt("dve", At[:], kkn[:], -1.0, E1[:], ALU.mult, ALU.mult, [bkkn, bE1], [bAt])
                    self.tt("dve", tA[:], kkn[:], a_[:], ALU.mult, [bkkn, ba, btA], [btA])
                    self.tt("dve", Bt[:], tA[:], E2[:], ALU.mult, [btA, bE2], [bBt])
                    self.tt("dve", Kt[:], k2_[:], E2[:], ALU.mult, [bk2, bE2], [bKt])
                    self.tt("dve", Rt[:], r_[:], E3[:], ALU.mult, [br, bE3], [bRt])
                    self.cp("act", vb[:], v_[:], [bv], [bvb])
                    self.tt("dve", tA[:], r_[:], k2_[:], ALU.mult, [br, bk2, btA], [btA])
                    for c in range(4):
                        self.ts("dve", sqk[:, c, :], tA[:, c, :], V["rwkv_r_k"][:, l, c:c + 1], None, ALU.mult, None, [btA, cb, bsqk], [bsqk])
                    for c in range(4):
                        ps, bps = pA.next()
                        self.mm(ps[:, 0:ST], self.bones[:, 0, :], sqk[:, c, :], True, True, [bsqk, cb], [bps])
                        self.tt("dve", bonus[:, c, :], ps[:, 0:ST], v_[:, c, :], ALU.mult, [bps, bv], [bbon])
                    tiles = {}

                    def prep(j):
                        sl = j % 2
                        c0 = j * 128
                        vt, bvt = Vtok.next(); bt, bbt = Btok.next(); kt, bkt = Ktok.next()
                        tiles[j] = (vt, bvt, bt, bbt, kt, bkt)
                        for (src, bsrc, dst, bdst) in ((vb, bvb, vt, bvt), (Bt, bBt, bt, bbt), (Kt, bKt, kt, bkt)):
                            for c in range(4):
                                self.tr(pTb[:, c * 128:(c + 1) * 128], src[:, c, c0:c0 + 128], self.identb[:], [bsrc, cb], [bpTb])
                            self.cp(self.evac_eng(), dst[:], pTb[:, 0:512], [bpTb], [bdst])
                        yield
                        jobs = ((Bt, bBt, At, bAt, 1, Nm, bN), (At, bAt, Bt, bBt, 2, NTm, bNT), (Kt, bKt, At, bAt, 1, LakT[sl], bLak[sl]),
                                (Bt, bBt, Rt, bRt, 0, LrbT[sl], bLrb[sl]), (Kt, bKt, Rt, bRt, 0, LrkT[sl], bLrk[sl]))
                        for ji, (lt, blt, rt, brt, mi, dst, bdst) in enumerate(jobs):
                            pw, bpw = wide(ji % 2)
                            for hidx in range(8):
                                par, q_ = hidx // 4, hidx % 4
                                rows = slice(par * 64, par * 64 + 64)
                                self.mm(pw[:, hidx * 128:(hidx + 1) * 128], lt[rows, q_, c0:c0 + 128], rt[rows, q_, c0:c0 + 128], True, True,
                                        [blt, brt], [bpw])
                            for half in range(2):
                                self.tt("dve", dst[:, half * 4:half * 4 + 4, :],
                                        pw[:, half * 512:(half + 1) * 512].rearrange("p (h t) -> p h t", h=4),
                                        self.masks[:, mi, :].rearrange("p (h t) -> p h t", h=4), ALU.mult, [bpw, cb], [bdst])
                            yield
                        for half in range(2):
                            self.tt("dve", Sm32[:, half * 4:half * 4 + 4, :], Nm[:, half * 4:half * 4 + 4, :],
                                    self.masks[:, 3, :].rearrange("p (h t) -> p h t", h=4), ALU.add, [bN, cb], [bSm[0]])
                        if NDT == F32:
                            Scur, bScur = Sm32, bSm[0]
                        else:
                            self.cp("act", Sm[0][:], Sm32[:], [bSm[0]], [bSm[0]])
                            Scur, bScur = Sm[0], bSm[0]
                        Mc, bMc, MTc, bMTc = Nm, bN, NTm, bNT
                        for step in range(6):
                            if step > 0:
                                pw, bpw = wide(0)
                                for h in range(8):
                                    self.mm(pw[:, h * 128:(h + 1) * 128], MTc[:, h, :], Scur[:, h, :], True, True, [bMTc, bScur], [bpw])
                                nxt = Sm[step % 2] if NDT != F32 else Sm32
                                bnxt = bSm[step % 2] if NDT != F32 else bSm[0]
                                if NDT == F32:
                                    self.tt("dve", Sm32[:].rearrange("p h t -> p (h t)"), pw[:], Sm32[:].rearrange("p h t -> p (h t)"), ALU.add,
                                            [bpw, bScur], [bnxt])
                                else:
                                    self.tt("dve", Sm32[:].rearrange("p h t -> p (h t)"), pw[:], Sm32[:].rearrange("p h t -> p (h t)"), ALU.add,
                                            [bpw, bSm[0], bSm[1]], [bSm[0], bSm[1]])
                                    self.cp("act", nxt[:], Sm32[:], [bSm[0], bSm[1]], [bnxt])
                                Scur, bScur = nxt, bnxt
                            if step < 5:
                                i = step % 2
                                pw, bpw = wide(1)
                                for h in range(8):
                                    self.mm(pw[:, h * 128:(h + 1) * 128], MTc[:, h, :], Mc[:, h, :], True, True, [bMTc, bMc], [bpw])
                                self.cp("act", Mm[i][:].rearrange("p h t -> p (h t)"), pw[:], [bpw], [bM[i]])
                                pw2, bpw2 = wide(0)
                                for h in range(8):
                                    self.mm(pw2[:, h * 128:(h + 1) * 128], Mc[:, h, :], MTc[:, h, :], True, True, [bMTc, bMc], [bpw2])
                                self.cp("act", MTm[i][:].rearrange("p h t -> p (h t)"), pw2[:], [bpw2], [bMT[i]])
                                Mc, bMc, MTc, bMTc = Mm[i], bM[i], MTm[i], bMT[i]
                            yield
                        self.cp("act", Zb[sl][:], Scur[:], [bScur], [bZ[sl]])

                    def seq(j):
                        nonlocal hp
                        sl = j % 2
                        c0 = j * 128
                        vt, bvt, bt, bbt, kt, bkt = tiles[j]
                        for par in range(2):
                            self.cp("pool", Vpad[:].rearrange("p (q w) c -> p q w c", w=2)[:, :, par, par * 64:(par + 1) * 64],
                                    vt[:].rearrange("p (q w c) -> p q w c", w=2, c=64)[:, :, par, :], [bvt], [bVp])
                        for ch in range(2):
                            rows = slice(ch * 64, ch * 64 + 64)
                            Hc32, Hcb, Hcd, bHc = H32[hp], Hbf[hp], Hbd[hp], bH[hp]
                            Hn32, Hnb, Hnd, bHn = H32[1 - hp], Hbf[1 - hp], Hbd[1 - hp], bH[1 - hp]
                            psx, bpsx = pA.next()
                            for h in range(8):
                                q_, par = h // 2, h % 2
                                hi = par * 4 + q_
                                self.mm(psx[:, h * 64:(h + 1) * 64], At[:, q_, c0:c0 + 128], Hcd[:, q_, par * 64:(par + 1) * 64], True, False, [bAt, bHc], [bpsx])
                                self.mm(psx[:, h * 64:(h + 1) * 64], LakT[sl][:, hi, :], vt[:, h * 64:(h + 1) * 64], False, True, [bLak[sl], bvt], [bpsx])
                            self.cp("act", Xsb[rows, :], psx[rows, :], [bpsx], [bX])
                            yield
                            psu, bpsu = pA.next()
                            for h in range(8):
                                hi = (h % 2) * 4 + h // 2
                                self.mm(psu[:, h * 64:(h + 1) * 64], Zb[sl][rows, hi, :], Xsb[rows, h * 64:(h + 1) * 64], True, True, [bZ[sl], bX], [bpsu])
                            self.cp("dve", Usb[rows, :], psu[rows, :], [bpsu], [bU])
                            for par in range(2):
                                self.cp("dve", Upad[rows].rearrange("p (q w) c -> p q w c", w=2)[:, :, par, par * 64:(par + 1) * 64],
                                        psu[rows, :].rearrange("p (q w c) -> p q w c", w=2, c=64)[:, :, par, :], [bpsu], [bUp])
                            yield
                            psh, bpsh = pA.next()
                            for h in range(8):
                                q_, par = h // 2, h % 2
                                o_ap = psh[:, par * 256 + q_ * 64:par * 256 + q_ * 64 + 64]
                                self.mm(o_ap, bt[rows, q_ * 128:(q_ + 1) * 128], Usb[rows, h * 64:(h + 1) * 64], True, False, [bbt, bU], [bpsh])
                                self.mm(o_ap, kt[rows, q_ * 128:(q_ + 1) * 128], vt[rows, h * 64:(h + 1) * 64], False, True, [bkt, bvt], [bpsh])
                            if ch == 0:
                                psy, bpsy = pY, bpY
                            for q_ in range(4):
                                o_ap = psy[:, q_ * 128 + ch * 64:q_ * 128 + ch * 64 + 64]
                                self.mm(o_ap, Hcd[:, q_, :], Rt[:, q_, c0 + ch * 64:c0 + ch * 64 + 64], True, False, [bHc, bRt], [bpsy])
                                for par in range(2):
                                    h = q_ * 2 + par
                                    hi = par * 4 + q_
                                    self.mm(o_ap, Upad[rows, h, :], LrbT[sl][rows, hi, ch * 64:ch * 64 + 64], False, False, [bUp, bLrb[sl]], [bpsy])
                                    self.mm(o_ap, Vpad[rows, h, :], LrkT[sl][rows, hi, ch * 64:ch * 64 + 64], False, par == 1, [bVp, bLrk[sl]], [bpsy])
                            yield
                            for par in range(2):
                                hr = slice(par * 64, par * 64 + 64)
                                self.tt("dve", htmp[hr].rearrange("p q v -> p (q v)"), Hc32[hr].rearrange("p q v -> p (q v)"),
                                        psh[hr, par * 256:(par + 1) * 256], ALU.add, [bHc, bpsh], [bht])
                            wc = E3[:, :, c0 + ch * 64 + 63:c0 + ch * 64 + 64].to_broadcast([128, 4, 64])
                            for par in range(2):
                                hr = slice(par * 64, par * 64 + 64)
                                self.tt("dve", Hnd[hr, :, par * 64:(par + 1) * 64], htmp[hr],
                                        E3[hr, :, c0 + ch * 64 + 63:c0 + ch * 64 + 64].to_broadcast([64, 4, 64]), ALU.mult, [bht, bE3], [bHn])
                            self.tt("dve", Hn32[:], htmp[:], wc, ALU.mult, [bht, bE3], [bHn])
                            hp = 1 - hp
                            yield
                        self.cp("act", yT[:, :, c0:c0 + 128], psy[:].rearrange("p (q t) -> p q t", q=4), [bpsy], [byT])

                    def drain(*gens):
                        act_ = list(gens)
                        while act_:
                            for g_ in list(act_):
                                try:
                                    next(g_)
                                except StopIteration:
                                    act_.remove(g_)

                    drain(prep(0))
                    for j in range(NTL):
                        if j + 1 < NTL:
                            drain(seq(j), prep(j + 1))
                        else:
                            drain(seq(j))
                    if self.cstop < 6:
                        continue
                    self.cp("act", ybf[:], yT[:], [byT], [bybf])
                    for c in range(4):
                        ps, bps = pA.next()
                        self.mm(ps[:, 0:ST], self.bones[:, 1, :], ybf[:, c, :], True, True, [bybf, cb], [bps])
                        self.tt("dve", dd[:, c, :], yT[:, c, :], ps[:, 0:ST], ALU.subtract, [byT, bps], [bdd])
                    self.act(sq2[:], dd[:], AF.Square, [bdd], [bsq2])
                    for c in range(4):
                        ps, bps = pA.next()
                        self.mm(ps[:, 0:ST], self.bones[:, 1, :], sq2[:, c, :], True, True, [bsq2, cb], [bps])
                        self.act(rs2[:, c, :], ps[:, 0:ST], AF.Ln, [bps, cb], [brs2], bias=self.cst[:, 3:4])
                    self.act(rs2[:], rs2[:], AF.Exp, [brs2], [brs2], scale=-0.5)
                    self.tt("dve", dd[:], dd[:], rs2[:], ALU.mult, [bdd, brs2], [bdd])
                    for c in range(4):
                        self.act(dd[:, c, :], dd[:, c, :], AF.Identity, [bdd, cb], [bdd], bias=V["rwkv_lnx_b"][:, l, c:c + 1],
                                 scale=V["rwkv_lnx_g"][:, l, c:c + 1])
                    self.tt("dve", dd[:], dd[:], bonus[:], ALU.add, [bdd, bbon], [bdd])
                    self.tt("dve", yfin[:], dd[:], gg[:], ALU.mult, [bdd, bgg], [byfin])
                    for dc in range(8):
                        gbt, bgbt = gbr.next()
                        self.dma(gbt[:], pT_[GBOFF + dc * 128:GBOFF + (dc + 1) * 128, t0:t0 + ST], reads=[self.b_pT], writes=[bgbt])
                        yat, byat = yagr.next()
                        self.dma(yat[:], self.yaT[dc * 128:(dc + 1) * 128, t0:t0 + ST], reads=[self.b_yaT], writes=[byat])
                        self.act(gbt[:], gbt[:], AF.Sigmoid, [bgbt], [bgbt])
                        ps, bps = pA.next()
                        for k in range(4):
                            self.mm(ps[:, 0:ST], rwo[:, k, dc * 128:(dc + 1) * 128], yfin[:, k, :], k == 0, k == 3, [bw, byfin], [bps])
                        mt, bmt = mtmp.next()
                        self.tt("dve", mt[:], ps[:, 0:ST], gbt[:], ALU.mult, [bps, bgbt], [bmt])
                        self.tt("dve", merged[:, dc, :], mt[:], yat[:], ALU.add, [bmt, byat], [bmg])
                    for dc in range(8):
                        xt, bxt = xtr.next()
                        self.dma(xt[:], self.xT[dc * 128:(dc + 1) * 128, t0:t0 + ST], reads=[self.b_xT], writes=[bxt])
                        ps, bps = pA.next()
                        for k in range(8):
                            self.mm(ps[:, 0:ST], wout[:, k, dc * 128:(dc + 1) * 128], merged[:, k, :], k == 0, k == 7, [bw, bmg], [bps])
                        self.stt("dve", xt[:], ps[:, 0:ST], self.modT[:, l, 16 + dc, b:b + 1], xt[:], ALU.mult, ALU.add,
                                 [bps, cb, bxt], [bxt])
                        self.dma(self.xT[dc * 128:(dc + 1) * 128, t0:t0 + ST], xt[:], reads=[bxt], writes=[self.b_xT], q="act")
        P.barrier()

    def phase_F(self, l):
        P, din, NT, S = self.P, self.din, self.NT, self.S
        cb = self.bc
        moe = (l % 2 == 1)
        li = l // 2
        FD = MOE_FFN if moe else FFN_DIM
        nexp = NEXP if moe else 1
        TB = min(1024, NT)
        TA = min(512, S)
        G = 4
        for p0 in range(0, NT, TB):
            with contextlib.ExitStack() as st:
                hT = self.sb(st, [128, 8, TB], BF16, "fh"); bh = Buf()
                acc = self.sb(st, [128, 8, TB], F32, "facc"); bacc = [Buf() for _ in range(TB // 512)]
                combT = self.sb(st, [8, TB], F32, "combT"); bcomb = Buf()
                cber = self.ring(st, [128, TB], F32, 2, "cbe")
                cbe, bcbe = cber.next()
                with contextlib.ExitStack() as st2:
                    objs = self.norm_objs(st2, TA)
                    if moe:
                        h32 = self.sb(st2, [128, 8, TA], F32, "h32"); bh32 = Buf()
                        rt = self.sb(st2, [128, 8, NEXP], F32, "rt"); brt = Buf()
                        self.dma(rt[:], din["moe_router"][li].rearrange("(k p) e -> p k e", p=128), writes=[brt])
                        psl = self.ps(st2, [128, 512], F32, "psl"); bpsl = Buf()
                        lg = self.sb(st2, [128, 8], F32, "lg"); m8 = self.sb(st2, [128, 8], F32, "m8"); nm1 = self.sb(st2, [128, 1], F32, "nm1")
                        msk = self.sb(st2, [128, 8], F32, "msk"); ex = self.sb(st2, [128, 8], F32, "ex"); ssum = self.sb(st2, [128, 1], F32, "ssum")
                        cmb = self.sb(st2, [128, 8], F32, "cmb")
                        brl = Buf()
                    for t0 in range(p0, p0 + TB, TA):
                        if moe:
                            self.norm_tile(objs, l, 2, t0, TA, t0 // S, hT[:, :, t0 - p0:t0 - p0 + TA], bh, h32, bh32)
                            for j in range(TA // 128):
                                for k in range(8):
                                    self.mm(psl[:, 0:8], h32[:, k, j * 128:(j + 1) * 128], rt[:, k, :], k == 0, k == 7, [bh32, brt], [bpsl])
                                self.cp("dve", lg[:], psl[:, 0:8], [bpsl], [brl])
                                P.op("dve", lambda e: e.max(m8[:], lg[:]), [brl], [brl])
                                self.ts("dve", nm1[:], m8[:, 0:1], -1.0, None, ALU.mult, None, [brl], [brl])
                                self.ts("dve", msk[:], lg[:], m8[:, 1:2], None, ALU.is_ge, None, [brl], [brl])
                                self.act(ex[:], lg[:], AF.Exp, [brl], [brl], bias=nm1[:, 0:1])
                                self.tt("dve", ex[:], ex[:], msk[:], ALU.mult, [brl], [brl])
                                P.op("dve", lambda e: e.tensor_reduce(ssum[:], ex[:], mybir.AxisListType.X, ALU.add), [brl], [brl])
                                P.op("dve", lambda e: e.reciprocal(ssum[:], ssum[:]), [brl], [brl])
                                self.ts("dve", cmb[:], ex[:], ssum[:, 0:1], None, ALU.mult, None, [brl], [brl])
                                self.tr(psl[0:8, 128:256], cmb[:], self.identf[:], [brl, cb], [bpsl])
                                self.cp("dve", combT[:, t0 - p0 + j * 128:t0 - p0 + (j + 1) * 128], psl[0:8, 128:256], [bpsl], [bcomb])
                        else:
                            self.norm_tile(objs, l, 2, t0, TA, t0 // S, hT[:, :, t0 - p0:t0 - p0 + TA], bh)
                    P.barrier()
                P.op("pool", lambda e: e.memset(acc[:], 0.0), (), bacc)
                wst = self.ring(st, [128, 8, 256], F32, 5, "fwst")
                w1b = self.ring(st, [128, 8, G * 128], BF16, 2, "fw1")
                w3b = self.ring(st, [128, 8, G * 128], BF16, 2, "fw3")
                w2b = self.ring(st, [128, G, D], BF16, 3, "fw2")
                gp = self.ring(st, [128, G, 512], BF16, 3, "fgp")
                sil = self.ring(st, [128, 512], F32, 2, "fsil")
                tq = self.ring(st, [128, 512], F32, 2, "ftq")
                ps1 = self.psring(st, [128, 512], F32, 2, "fps1")
                ps3 = self.psring(st, [128, 512], F32, 2, "fps3")
                psy = self.psring(st, [128, 512], F32, 3, "fpsy")
                pscb = self.ps(st, [128, 512], F32, "fpscb"); bpscb = Buf()
                ce = 0
                pendY = []

                def emit_Y(a2, ba2, g_, bg_, ng, tt0, ti):
                    for dc in range(8):
                        py, bpy = psy.next()
                        for fi in range(ng):
                            self.mm(py[:], a2[:, fi, dc * 128:(dc + 1) * 128], g_[:, fi, :], fi == 0, fi == ng - 1, [ba2, bg_], [bpy])
                        self.tt("dve", acc[:, dc, tt0:tt0 + 512], py[:], acc[:, dc, tt0:tt0 + 512], ALU.add, [bpy, bacc[ti]], [bacc[ti]])

                groups = []
                for e_ in range(nexp):
                    for f0 in range(0, FD, G * 128):
                        groups.append((e_, f0))
                loaded = {}
                cbes = {}
                cnt_ce = [0]

                def load_group(gi):
                    e_, f0 = groups[gi]
                    if moe:
                        W1, W3, W2 = din["moe_w1"][li * NEXP + e_], din["moe_w3"][li * NEXP + e_], din["moe_w2"][li * NEXP + e_]
                    else:
                        W1, W3, W2 = din["ffn_w1"][li], din["ffn_w3"][li], din["ffn_w2"][li]
                    nf = min(G * 128, FD - f0)
                    ng = nf // 128
                    a1, ba1 = w1b.next(); a3, ba3 = w3b.next(); a2, ba2 = w2b.next()
                    for (Wsrc, dstt, bdst) in ((W1, a1, ba1), (W3, a3, ba3)):
                        for hf in range(0, nf, 256):
                            w, bws = wst.next()
                            self.dma(w[:], Wsrc[:, f0 + hf:f0 + hf + 256].rearrange("(k p) n -> p k n", p=128), writes=[bws])
                            cnt_ce[0] += 1
                            self.cp("act" if cnt_ce[0] % 3 else "pool", dstt[:, :, hf:hf + 256], w[:], [bws], [bdst])
                    for hf in range(0, ng, 2):
                        w, bws = wst.next()
                        wv = w[:].rearrange("p k n -> p (k n)").rearrange("p (f d) -> p f d", f=2)
                        self.dma(wv, W2[f0 + hf * 128:f0 + (hf + 2) * 128, :].rearrange("(f p) d -> p f d", p=128), writes=[bws])
                        cnt_ce[0] += 1
                        self.cp("act" if cnt_ce[0] % 3 else "pool", a2[:, hf:hf + 2, :], wv, [bws], [ba2])
                    loaded[gi] = (a1, ba1, a3, ba3, a2, ba2, ng)
                    if moe and e_ not in cbes:
                        cbe, bcbe = cber.next()
                        for tt0 in range(0, TB, 512):
                            self.mm(pscb[:], self.sel[:, e_, :], combT[:, tt0:tt0 + 512], True, True, [bcomb, cb], [bpscb])
                            self.cp("act", cbe[:, tt0:tt0 + 512], pscb[:], [bpscb], [bcbe])
                        cbes[e_] = (cbe, bcbe)

                load_group(0)
                for gi, (e_, f0) in enumerate(groups):
                    if gi + 1 < len(groups):
                        load_group(gi + 1)
                    a1, ba1, a3, ba3, a2, ba2, ng = loaded.pop(gi)
                    if moe:
                        cbe, bcbe = cbes[e_]
                    if True:
                        for ti, tt0 in enumerate(range(0, TB, 512)):
                            g_, bg_ = gp.next()
                            for fi in range(ng):
                                p1, bp1 = ps1.next(); p3, bp3 = ps3.next()
                                for k in range(8):
                                    self.mm(p1[:], a1[:, k, fi * 128:(fi + 1) * 128], hT[:, k, tt0:tt0 + 512], k == 0, k == 7, [ba1, bh], [bp1])
                                for k in range(8):
                                    self.mm(p3[:], a3[:, k, fi * 128:(fi + 1) * 128], hT[:, k, tt0:tt0 + 512], k == 0, k == 7, [ba3, bh], [bp3])
                                s_, bs_ = sil.next()
                                self.act(s_[:], p1[:], AF.Silu, [bp1], [bs_])
                                if moe:
                                    q_, bq_ = tq.next()
                                    self.tt("dve", q_[:], p3[:], s_[:], ALU.mult, [bp3, bs_], [bq_])
                                    self.tt("dve", g_[:, fi, :], q_[:], cbe[:, tt0:tt0 + 512], ALU.mult, [bq_, bcbe], [bg_])
                                else:
                                    self.tt("dve", g_[:, fi, :], p3[:], s_[:], ALU.mult, [bp3, bs_], [bg_])
                            if pendY:
                                emit_Y(*pendY.pop())
                            pendY.append((a2, ba2, g_, bg_, ng, tt0, ti))
                if pendY:
                    emit_Y(*pendY.pop())
                with contextlib.ExitStack() as st3:
                    xr = self.ring(st3, [128, TA], F32, 3, "fxr")
                    for t0 in range(p0, p0 + TB, TA):
                        b = t0 // S
                        for dc in range(8):
                            xt, bxt = xr.next()
                            self.dma(xt[:], self.xT[dc * 128:(dc + 1) * 128, t0:t0 + TA], reads=[self.b_xT], writes=[bxt])
                            self.stt("dve", xt[:], acc[:, dc, t0 - p0:t0 - p0 + TA], self.modT[:, l, 40 + dc, b:b + 1],
                                     xt[:], ALU.mult, ALU.add, [bacc[(t0 - p0) // 512], cb, bxt], [bxt])
                            self.dma(self.xT[dc * 128:(dc + 1) * 128, t0:t0 + TA], xt[:], reads=[bxt], writes=[self.b_xT], q="act")
            P.barrier()

    def final(self):
        P, NT = self.P, self.NT
        cb = self.bc
        with contextlib.ExitStack() as st:
            xr = self.ring(st, [128, 8, 512], F32, 2, "zx")
            sq = self.sb(st, [128, 8, 512], BF16, "zsq"); bsq = Buf()
            psn = self.ps(st, [128, 512], F32, "zpsn"); bpsn = Buf()
            rstd = self.sb(st, [128, 512], F32, "zrs"); brs = Buf()
            yv = self.sb(st, [128, 8, 512], F32, "zy"); by = Buf()
            pst = self.psring(st, [128, 512], F32, 4, "zpst")
            ot = self.ring(st, [128, D], F32, 3, "zot")
            for t0 in range(0, NT, 512):
                xt, bx = xr.next()
                self.dma(xt[:], self.xT[:, t0:t0 + 512].rearrange("(c p) t -> p c t", p=128), reads=[self.b_xT], writes=[bx])
                self.act(sq[:], xt[:], AF.Square, [bx], [bsq])
                for k in range(8):
                    self.mm(psn[:], self.onesD[:], sq[:, k, :], k == 0, k == 7, [bsq, cb], [bpsn])
                self.act(rstd[:], psn[:], AF.Ln, [bpsn, cb], [brs], bias=self.cst[:, 1:2])
                self.act(rstd[:], rstd[:], AF.Exp, [brs], [brs], scale=-0.5)
                for k in range(8):
                    self.stt("dve", yv[:, k, :], xt[:, k, :], self.vec["lnf_g"][:, 0, k:k + 1], rstd[:], ALU.mult, ALU.mult,
                             [bx, brs, cb], [by])
                for j in range(4):
                    o, bo = ot.next()
                    for half in range(2):
                        ps, bps = pst.next()
                        for q in range(4):
                            k = half * 4 + q
                            self.tr(ps[:, q * 128:(q + 1) * 128], yv[:, k, j * 128:(j + 1) * 128], self.identf[:], [by, cb], [bps])
                        self.cp(self.evac_eng(), o[:, half * 512:(half + 1) * 512], ps[:], [bps], [bo])
                    self.dma(self.out[t0 + j * 128:t0 + (j + 1) * 128, :], o[:], reads=[bo], q="act")
        P.barrier()

    def build(self, do_mixer=True, do_ffn=True, phases="ABC"):
        self.declare()
        with contextlib.ExitStack() as st:
            self.setup_consts(st)
            self.prologue()
            for l in range(self.L):
                if do_mixer:
                    if "A" in phases:
                        self.phase_A(l)
                    if "B" in phases:
                        self.phase_B(l)
                    if "C" in phases:
                        self.phase_C(l)
                if do_ffn:
                    self.phase_F(l)
            self.final()
        self.P.emit()
        return self.nc


def host_consts():
    ident = np.eye(128, dtype=np.float32)
    s = np.arange(128)[:, None]
    t = np.arange(128)[None, :]
    same = (s // 64) == (t // 64)
    m_iu = (same & (s <= t)).astype(np.float32)
    m_su = (same & (s < t)).astype(np.float32)
    m_sl = (same & (s > t)).astype(np.float32)
    masks = np.zeros((128, 5, 512), np.float32)
    for i, m in enumerate([m_iu, m_su, m_sl, ident]):
        masks[:, i, :] = np.tile(m, (1, 4))
    bones = np.zeros((128, 2, 128), np.float32)
    blk = ((s // 64) == (t // 64)).astype(np.float32)
    bones[:, 0, :] = blk
    bones[:, 1, :] = blk / 64.0
    sel = np.zeros((8, 8, 128), np.float32)
    for e in range(8):
        sel[e, e, :] = 1.0
    scanm = np.ones((128, 1024), np.float32)
    scanm[:, ::64] = 0.0
    return dict(k_ident=ident, k_masks=masks, k_bones=bones, k_sel=sel, k_scanm=scanm)


_CACHE = {}


def run(inputs, S, L, n_cores, **bk):
    key = (S, L, tuple(sorted(bk.items())))
    if key not in _CACHE:
        kb = KB(S, L, neu_fp32=bk.pop("neu_fp32", True), debug=bk.pop("debug", False))
        kb.cstop = bk.pop("cstop", 9)
        _CACHE[key] = kb.build(**bk)
    nc = _CACHE[key]
    hc = host_consts()
    x = np.ascontiguousarray(inputs["x"], dtype=np.float32)
    in_maps = []
    shared = {}
    for k, v in inputs.items():
        if k in ("x", "c"):
            continue
        a = np.ascontiguousarray(v, dtype=np.float32)
        if k in ("moe_w1", "moe_w3", "moe_w2"):
            a = a.reshape((-1,) + a.shape[2:])
        if k == "rwkv_r_k":
            a = a.reshape(a.shape[0], 512)
        shared[k] = a
    shared.update(hc)
    for i in range(n_cores):
        m = dict(shared)
        m["x"] = x[2 * i:2 * i + 2].reshape(2 * S, D)
        m["c"] = np.ascontiguousarray(inputs["c"][2 * i:2 * i + 2], dtype=np.float32)
        in_maps.append(m)
    import os as _os
    if _os.environ.get("KTRACE"):
        res = run_bass_kernel_spmd(nc, in_maps, core_ids=list(range(n_cores)), trace=True)
        print("EXEC_TIME_NS", res.exec_time_ns)
    else:
        res = run_bass_kernel_spmd(nc, in_maps, core_ids=list(range(n_cores)))
    global LAST
    LAST = res.results
    out = np.stack([r["out"].reshape(2, S, D) for r in res.results], axis=0)
    return out.reshape(2 * n_cores, S, D).astype(np.float32)


def kernel(**inputs):
    return run(inputs, 2048, 4, 8)
```

```python
import contextlib
import numpy as np
import concourse.bass as bass
import concourse.mybir as mybir
from concourse.bass_utils import run_bass_kernel_spmd

F32 = mybir.dt.float32
BF16 = mybir.dt.bfloat16
AF = mybir.ActivationFunctionType
ALU = mybir.AluOpType

ENGS = ("pe", "act", "dve", "pool", "sp")
D = 1024
N_IN = 6928
FFN_DIM = 2816
MOE_FFN = 3584
NEXP = 8
ROFF = 3088
GAOFF = 4880
GBOFF = 5904
EXPM05 = 0.6065306597126334


class Buf:
    __slots__ = ("w", "r")

    def __init__(self):
        self.w = None
        self.r = {}


class Prog:
    def __init__(self, nc, n_dma_sems=32, same_engine_sync=True):
        self.nc = nc
        self.streams = {e: [] for e in ENGS}
        self.cnt = {}
        self.seen = {e: {} for e in ENGS}
        self.semnames = list(ENGS[:4]) + ["dma%d" % i for i in range(n_dma_sems)]
        for k in self.semnames:
            self.cnt[k] = 0
        self.same_engine_sync = same_engine_sync
        self.n_dma = n_dma_sems
        self.dma_rr = 0
        self.n_instr = 0
        self.n_wait = 0
        self.pending = {e: False for e in ENGS}
        self.snap_n = {e: [] for e in ENGS[:4]}
        self.snap_d = {e: [] for e in ENGS[:4]}
        self.dirty = {e: True for e in ENGS}

    def _inherit(self, eng, key, val):
        ns = self.snap_n.get(key)
        if not ns:
            return
        import bisect
        i = bisect.bisect_right(ns, val) - 1
        if i < 0:
            return
        se = self.seen[eng]
        for k2, v2 in self.snap_d[key][i].items():
            if k2 != eng and se.get(k2, 0) < v2:
                se[k2] = v2

    def _need(self, eng, deps):
        for key, val in deps.items():
            if val <= 0:
                continue
            if key == eng and (eng == "pe" or not self.same_engine_sync):
                continue
            if self.seen[eng].get(key, 0) >= val:
                continue
            self.seen[eng][key] = val
            self.dirty[eng] = True
            self.streams[eng].append(("wait", key, val))
            self.n_wait += 1
            if key != eng:
                self._inherit(eng, key, val)

    @staticmethod
    def _collect(reads, writes):
        deps = {}
        for b in reads:
            if b.w is not None and deps.get(b.w[0], 0) < b.w[1]:
                deps[b.w[0]] = b.w[1]
        for b in writes:
            if b.w is not None and deps.get(b.w[0], 0) < b.w[1]:
                deps[b.w[0]] = b.w[1]
            for k, v in b.r.items():
                if deps.get(k, 0) < v:
                    deps[k] = v
        return deps

    def op(self, eng, fn, reads=(), writes=(), inc=True):
        self._need(eng, self._collect(reads, writes))
        if inc:
            self.cnt[eng] += 1
            v = self.cnt[eng]
            self.pending[eng] = False
            if self.dirty[eng]:
                self.snap_n[eng].append(v)
                self.snap_d[eng].append(dict(self.seen[eng]))
                self.dirty[eng] = False
        else:
            v = self.cnt[eng] + 1
            self.pending[eng] = True
        self.streams[eng].append(("op", fn, eng, 1 if inc else 0))
        self.n_instr += 1
        for b in reads:
            if b.r.get(eng, 0) < v:
                b.r[eng] = v
        for b in writes:
            b.w = (eng, v)
            b.r = {}

    def dma(self, fn, reads=(), writes=(), q="sp"):
        self._need(q, self._collect(reads, writes))
        semkey = "dma%d" % self.dma_rr
        self.dma_rr = (self.dma_rr + 1) % self.n_dma
        self._need(q, {semkey: self.cnt[semkey]})
        self.cnt[semkey] += 16
        v = self.cnt[semkey]
        self.streams[q].append(("op", fn, semkey, 16))
        self.n_instr += 1
        for b in reads:
            if b.r.get(semkey, 0) < v:
                b.r[semkey] = v
        for b in writes:
            b.w = (semkey, v)
            b.r = {}

    def barrier(self):
        assert not any(self.pending.values()), self.pending
        deps = dict(self.cnt)
        for e in ENGS:
            self._need(e, deps)

    def emit(self):
        nc = self.nc
        with contextlib.ExitStack() as st:
            sems = {k: st.enter_context(nc.semaphore("s_" + k)) for k in self.semnames}
            block = st.enter_context(nc.Block())

            def run(stream, engobj):
                for it in stream:
                    if it[0] == "wait":
                        engobj.wait_ge(sems[it[1]], it[2])
                    elif it[3]:
                        it[1](engobj).then_inc(sems[it[2]], it[3])
                    else:
                        it[1](engobj)

            block.tensor(lambda e: run(self.streams["pe"], e))
            block.scalar(lambda e: run(self.streams["act"], e))
            block.vector(lambda e: run(self.streams["dve"], e))
            block.gpsimd(lambda e: run(self.streams["pool"], e))
            block.sync(lambda e: run(self.streams["sp"], e))


class Ring:
    def __init__(self, tiles):
        self.tiles = tiles
        self.bufs = [Buf() for _ in tiles]
        self.i = 0

    def next(self):
        t, b = self.tiles[self.i], self.bufs[self.i]
        self.i = (self.i + 1) % len(self.tiles)
        return t, b


class KB:
    def __init__(self, S, L, neu_fp32=True, debug=False):
        self.debug = debug
        self.cstop = 9
        self.S, self.L = S, L
        self.NB = 2
        self.NT = 2 * S
        self.neu_dt = F32 if neu_fp32 else BF16
        self.nc = bass.Bass("TRN2", target_bir_lowering=False)
        import os as _os2
        self.P = Prog(self.nc, same_engine_sync=not _os2.environ.get("NOSES"))
        self.uid = 0
        self.rr = 0

    def name(self, s):
        self.uid += 1
        return "%s_%d" % (s, self.uid)

    def sb(self, st, shape, dt=F32, name="t"):
        return st.enter_context(self.nc.sbuf_tensor(self.name(name), list(shape), dt))

    def ps(self, st, shape, dt=F32, name="p"):
        return st.enter_context(self.nc.psum_tensor(self.name(name), list(shape), dt))

    def ring(self, st, shape, dt, n, name="r"):
        return Ring([self.sb(st, shape, dt, name) for _ in range(n)])

    def psring(self, st, shape, dt, n, name="pr"):
        return Ring([self.ps(st, shape, dt, name) for _ in range(n)])

    def mm(self, out, lhsT, rhs, start, stop, reads, writes):
        self.P.op("pe", lambda e: e.matmul(out, lhsT, rhs, start=start, stop=stop), reads, writes, inc=bool(stop))

    def tr(self, out, in_, ident, reads, writes):
        self.P.op("pe", lambda e: e.transpose(out, in_, ident), reads, writes)

    def act(self, out, in_, func, reads, writes, bias=None, scale=None):
        kw = {}
        if bias is not None:
            kw["bias"] = bias
        if scale is not None:
            kw["scale"] = scale
        self.P.op("act", lambda e: e.activation(out, in_, func, **kw), reads, writes)

    def tt(self, eng, out, a, b, op, reads, writes):
        self.P.op(eng, lambda e: e.tensor_tensor(out, a, b, op), reads, writes)

    def ts(self, eng, out, a, s1, s2, op0, op1, reads, writes):
        if s2 is None:
            self.P.op(eng, lambda e: e.tensor_scalar(out, a, s1, None, op0), reads, writes)
        else:
            self.P.op(eng, lambda e: e.tensor_scalar(out, a, s1, s2, op0, op1), reads, writes)

    def stt(self, eng, out, a, s, b, op0, op1, reads, writes):
        self.P.op(eng, lambda e: e.scalar_tensor_tensor(out, a, s, b, op0, op1), reads, writes)

    def cp(self, eng, out, in_, reads, writes):
        if eng == "act":
            self.P.op("act", lambda e: e.copy(out, in_), reads, writes)
        else:
            self.P.op(eng, lambda e: e.tensor_copy(out, in_), reads, writes)

    def dma(self, out, in_, reads=(), writes=(), slow=False, q="sp"):
        scr = (self.b_xT, self.b_pT, self.b_yaT)
        reads = [r for r in reads if r not in scr]
        writes = [w for w in writes if w not in scr]
        if slow:
            self.P.dma(lambda e: e.dma_start(out=out, in_=in_, allow_slow_non_contiguous=True), reads, writes, q=q)
        else:
            self.P.dma(lambda e: e.dma_start(out=out, in_=in_), reads, writes, q=q)

    def evac_eng(self):
        self.rr += 1
        return "act" if self.rr % 2 else "dve"

    def declare(self):
        nc, L, NT = self.nc, self.L, self.NT
        ND, NM = (L + 1) // 2, L // 2
        shapes = {
            "x": [NT, D], "c": [2, D], "w_ada": [L, D, 6 * D], "b_ada": [L, 6 * D], "ln1_g": [L, D], "ln2_g": [L, D],
            "w_in": [L, D, N_IN], "gla_alpha_up": [L, 16, 512], "gla_alpha_b": [L, 512], "gla_norm_g": [L, D],
            "gla_w_o": [L, D, D], "rwkv_mu": [L, 1792], "rwkv_w0": [L, 512], "rwkv_w2": [L, 64, 512],
            "rwkv_a0": [L, 512], "rwkv_a2": [L, 64, 512], "rwkv_g2": [L, 128, 512], "rwkv_k_k": [L, 512],
            "rwkv_k_a": [L, 512], "rwkv_r_k": [L, 512], "rwkv_lnx_g": [L, 512], "rwkv_lnx_b": [L, 512],
            "rwkv_w_o": [L, 512, D], "w_out": [L, D, D], "ffn_w1": [max(ND, 1), D, FFN_DIM], "ffn_w3": [max(ND, 1), D, FFN_DIM],
            "ffn_w2": [max(ND, 1), FFN_DIM, D], "moe_router": [max(NM, 1), D, NEXP],
            "moe_w1": [max(NM, 1) * NEXP, D, MOE_FFN], "moe_w3": [max(NM, 1) * NEXP, D, MOE_FFN],
            "moe_w2": [max(NM, 1) * NEXP, MOE_FFN, D], "lnf_g": [D],
            "k_ident": [128, 128], "k_masks": [128, 5, 512], "k_bones": [128, 2, 128], "k_sel": [8, 8, 128],
            "k_scanm": [128, 1024],
        }
        self.din = {k: nc.dram_tensor(k, v, F32, kind="ExternalInput").ap() for k, v in shapes.items()}
        self.in_shapes = shapes
        self.out = nc.dram_tensor("out", [NT, D], F32, kind="ExternalOutput").ap()
        kd = dict(kind="ExternalOutput") if self.debug else {}
        self.xT = nc.dram_tensor("xT_scr", [D, NT], F32, **kd).ap()
        self.pT = nc.dram_tensor("pT_scr", [N_IN, NT], F32, **kd).ap()
        self.yaT = nc.dram_tensor("yaT_scr", [D, NT], F32, **kd).ap()
        self.mod_scr = nc.dram_tensor("mod_scr", [L, 2, 6 * D], F32).ap()
        self.b_xT, self.b_pT, self.b_yaT, self.b_mod = Buf(), Buf(), Buf(), Buf()

    def setup_consts(self, st):
        P, din, L = self.P, self.din, self.L
        self.identf = self.sb(st, [128, 128], F32, "identf")
        self.identb = self.sb(st, [128, 128], BF16, "identb")
        self.masks = self.sb(st, [128, 5, 512], F32, "masks")
        self.bonesf = self.sb(st, [128, 2, 128], F32, "bonesf")
        self.bones = self.sb(st, [128, 2, 128], BF16, "bones")
        self.onesD = self.sb(st, [128, 128], BF16, "onesD")
        self.ones256 = self.sb(st, [128, 128], BF16, "ones256")
        self.sel = self.sb(st, [8, 8, 128], F32, "sel")
        self.scanm = self.sb(st, [128, 1024], F32, "scanm")
        self.cst = self.sb(st, [128, 8], F32, "cst")
        self.bc = Buf()
        cb = [self.bc]
        self.dma(self.identf[:], din["k_ident"], writes=cb)
        self.dma(self.masks[:], din["k_masks"], writes=cb)
        self.dma(self.bonesf[:], din["k_bones"], writes=cb)
        self.dma(self.sel[:], din["k_sel"], writes=cb)
        self.dma(self.scanm[:], din["k_scanm"], writes=cb)
        self.cp("dve", self.identb[:], self.identf[:], cb, cb)
        self.cp("dve", self.bones[:], self.bonesf[:], cb, cb)
        P.op("pool", lambda e: e.memset(self.onesD[:], 1.0 / 1024), (), cb)
        P.op("pool", lambda e: e.memset(self.ones256[:], 1.0 / 256), (), cb)
        for i, v in enumerate([1.0, 1e-6, 1e-5, 64e-5, 1e-24, 0.0]):
            P.op("pool", lambda e, i=i, v=v: e.memset(self.cst[:, i:i + 1], v), (), cb)
        self.vec = {}

        def colvec(key, n, nl=L):
            t = self.sb(st, [128, nl, n // 128], F32, "v_" + key)
            for l in range(nl):
                src = din[key][l] if nl > 1 or len(self.in_shapes[key]) == 2 else din[key]
                for c in range(n // 128):
                    self.dma(t[:, l, c:c + 1], src[c * 128:(c + 1) * 128].rearrange("(p o) -> p o", o=1), writes=[Buf()], slow=True, q="pool")
            self.vec[key] = t
        for key, n in [("ln1_g", D), ("ln2_g", D), ("gla_alpha_b", 512), ("gla_norm_g", D), ("rwkv_w0", 512),
                       ("rwkv_a0", 512), ("rwkv_k_k", 512), ("rwkv_k_a", 512), ("rwkv_r_k", 512),
                       ("rwkv_lnx_g", 512), ("rwkv_lnx_b", 512)]:
            colvec(key, n)
        t = self.sb(st, [128, 1, 8], F32, "v_lnf")
        for c in range(8):
            self.dma(t[:, 0, c:c + 1], din["lnf_g"][c * 128:(c + 1) * 128].rearrange("(p o) -> p o", o=1), writes=[Buf()], slow=True, q="pool")
        self.vec["lnf_g"] = t
        pieces = [(0, 128), (128, 128), (256, 128), (384, 128), (512, 64), (576, 128), (704, 128), (832, 128), (960, 128),
                  (1088, 128), (1216, 128), (1344, 128), (1472, 128), (1600, 64), (1664, 128)]
        self.mu_piece = {}
        mu = self.sb(st, [128, L, 15], F32, "v_mu2")
        for l in range(L):
            for i, (o, n) in enumerate(pieces):
                self.dma(mu[0:n, l, i:i + 1], din["rwkv_mu"][l][o:o + n].rearrange("(p o) -> p o", o=1), writes=[Buf()], slow=True, q="pool")
        self.vec["mu"] = mu
        nab = self.sb(st, [128, L, 4], F32, "v_nab")
        self.vec["neg_alpha_b"] = nab
        self.modT = self.sb(st, [128, L, 48, 2], F32, "modT")
        self.sc1 = self.sb(st, [128, L, 8, 2], F32, "sc1")
        self.sc2 = self.sb(st, [128, L, 8, 2], F32, "sc2")

    def prologue(self):
        P, din, L, NT = self.P, self.din, self.L, self.NT
        cb = [self.bc]
        with contextlib.ExitStack() as st:
            cT = self.sb(st, [128, 8, 2], F32, "cT")
            bcT = Buf()
            for b in range(2):
                for k in range(8):
                    self.dma(cT[:, k, b:b + 1], din["c"][b, k * 128:(k + 1) * 128].rearrange("(p o) -> p o", o=1), writes=[bcT], slow=True)
            self.act(cT[:], cT[:], AF.Silu, [bcT], [bcT])
            wst = self.ring(st, [128, 8, 512], F32, 4, "wada")
            psr = self.psring(st, [128, 512], F32, 2, "psada")
            modsb = self.sb(st, [2, 6 * D], F32, "modsb")
            bada = self.sb(st, [2, 6 * D], F32, "bada")
            bmod, bbada = Buf(), Buf()
            for l in range(L):
                self.dma(bada[:], din["b_ada"][l:l + 1, :].partition_broadcast(2), writes=[bbada])
                for g in range(12):
                    w, bw = wst.next()
                    self.dma(w[:], din["w_ada"][l][:, g * 512:(g + 1) * 512].rearrange("(k p) n -> p k n", p=128), writes=[bw],
                             q="sp" if g % 2 == 0 else "act")
                    ps, bps = psr.next()
                    for k in range(8):
                        self.mm(ps[0:2, :], cT[:, k, :], w[:, k, :], k == 0, k == 7, [bcT, bw], [bps])
                    self.tt("dve", modsb[:, g * 512:(g + 1) * 512], ps[0:2, :], bada[:, g * 512:(g + 1) * 512], ALU.add,
                            [bps, bbada], [bmod])
                bml = Buf()
                self.dma(self.mod_scr[l], modsb[:], reads=[bmod], writes=[bml], q="act")
                for b in range(2):
                    for cch in range(48):
                        self.dma(self.modT[:, l, cch, b:b + 1],
                                 self.mod_scr[l, b, cch * 128:(cch + 1) * 128].rearrange("(p o) -> p o", o=1),
                                 reads=[bml], writes=[Buf()], slow=True, q="pool")
            P.barrier()
            self.ts("dve", self.vec["neg_alpha_b"][:], self.vec["gla_alpha_b"][:], -1.0, None, ALU.mult, None, cb, cb)
            for l in range(L):
                for b in range(2):
                    self.stt("dve", self.sc1[:, l, :, b], self.modT[:, l, 8:16, b], 1.0, self.vec["ln1_g"][:, l, :], ALU.add, ALU.mult, cb, cb)
                    self.stt("dve", self.sc2[:, l, :, b], self.modT[:, l, 32:40, b], 1.0, self.vec["ln2_g"][:, l, :], ALU.add, ALU.mult, cb, cb)
        P.barrier()
        with contextlib.ExitStack() as st:
            xin = self.ring(st, [128, D], F32, 3, "xin")
            pst = self.psring(st, [128, 512], F32, 4, "pst")
            xo = self.ring(st, [128, 8, 512], F32, 2, "xo")
            for t0 in range(0, NT, 512):
                o, bo = xo.next()
                for j in range(4):
                    xi, bxi = xin.next()
                    self.dma(xi[:], din["x"][t0 + j * 128: t0 + (j + 1) * 128, :], writes=[bxi])
                    for half in range(2):
                        ps, bps = pst.next()
                        for q in range(4):
                            cch = half * 4 + q
                            self.tr(ps[:, q * 128:(q + 1) * 128], xi[:, cch * 128:(cch + 1) * 128], self.identf[:], [bxi, self.bc], [bps])
                        self.cp(self.evac_eng(), o[:, half * 4:(half + 1) * 4, j * 128:(j + 1) * 128],
                                ps[:].rearrange("p (q t) -> p q t", q=4), [bps], [bo])
                self.dma(self.xT[:, t0:t0 + 512].rearrange("(c p) t -> p c t", p=128), o[:], reads=[bo], writes=[self.b_xT], q="act")
        P.barrier()

    def norm_tile(self, st_objs, l, which, t0, TA, b, h_out, bh, h32_out=None, bh32=None):
        xt, bx, sq, bsq, psn, bpsn, rstd, brs, tmp, btmp = st_objs
        scale = (self.sc1 if which == 1 else self.sc2)
        shift_off = 0 if which == 1 else 24
        cb = self.bc
        self.dma(xt[:, :, 0:TA], self.xT[:, t0:t0 + TA].rearrange("(c p) t -> p c t", p=128), reads=[self.b_xT], writes=[bx])
        self.act(sq[:, :, 0:TA], xt[:, :, 0:TA], AF.Square, [bx], [bsq])
        for k in range(8):
            self.mm(psn[:, 0:TA], self.onesD[:], sq[:, k, 0:TA], k == 0, k == 7, [bsq, cb], [bpsn])
        self.act(rstd[:, 0:TA], psn[:, 0:TA], AF.Ln, [bpsn, cb], [brs], bias=self.cst[:, 1:2])
        self.act(rstd[:, 0:TA], rstd[:, 0:TA], AF.Exp, [brs], [brs], scale=-0.5)
        for k in range(8):
            eng = "dve" if k % 2 == 0 else "pool"
            self.tt(eng, tmp[:, k, 0:TA], xt[:, k, 0:TA], rstd[:, 0:TA], ALU.mult, [bx, brs], [btmp])
        for k in range(8):
            if h32_out is not None:
                self.act(h32_out[:, k, 0:TA], tmp[:, k, 0:TA], AF.Identity, [btmp, cb], [bh32],
                         bias=self.modT[:, l, shift_off + k, b:b + 1], scale=scale[:, l, k, b:b + 1])
                self.cp("pool", h_out[:, k, :], h32_out[:, k, 0:TA], [bh32], [bh])
            else:
                self.act(h_out[:, k, :], tmp[:, k, 0:TA], AF.Identity, [btmp, cb], [bh],
                         bias=self.modT[:, l, shift_off + k, b:b + 1], scale=scale[:, l, k, b:b + 1])

    def norm_objs(self, st, TA):
        xt = self.sb(st, [128, 8, TA], F32, "nx")
        sq = self.sb(st, [128, 8, TA], BF16, "nsq")
        psn = self.ps(st, [128, 512], F32, "npsn")
        rstd = self.sb(st, [128, TA], F32, "nrstd")
        tmp = self.sb(st, [128, 8, TA], F32, "ntmp")
        return (xt, Buf(), sq, Buf(), psn, Buf(), rstd, Buf(), tmp, Buf())

    def phase_A(self, l):
        P, din, NT, S = self.P, self.din, self.NT, self.S
        TA = min(512, S)
        with contextlib.ExitStack() as st:
            hT = self.sb(st, [128, 8, NT], BF16, "hT")
            bhs = [Buf() for _ in range(NT // TA)]
            objs = self.norm_objs(st, TA)
            wst = self.ring(st, [128, 8, 512], F32, 2, "wst")
            wbf = self.ring(st, [128, 8, 512], BF16, 2, "wbf")
            psr = self.psring(st, [128, 512], F32, 4, "psA")
            ost = self.ring(st, [128, 512], F32, 4, "ost")

            def load_chunk(c0):
                ncol = min(512, N_IN - c0)
                w, bw = wst.next()
                self.dma(w[:, :, 0:ncol], din["w_in"][l][:, c0:c0 + ncol].rearrange("(k p) n -> p k n", p=128), writes=[bw])
                wb, bwb = wbf.next()
                for k in range(8):
                    self.cp("pool" if k % 2 else "act", wb[:, k, 0:ncol], w[:, k, 0:ncol], [bw], [bwb])
                return wb, bwb, ncol

            def compute(c0, wb, bwb, ncol, t0):
                for cc in range(0, ncol, 128):
                    m = min(128, ncol - cc)
                    ps, bps = psr.next()
                    for k in range(8):
                        self.mm(ps[0:m, :], wb[:, k, cc:cc + m], hT[:, k, t0:t0 + 512], k == 0, k == 7,
                                [bwb] + bhs[t0 // TA:(t0 + 512) // TA], [bps])
                    o, bo = ost.next()
                    self.cp(self.evac_eng(), o[0:m, :], ps[0:m, :], [bps], [bo])
                    self.dma(self.pT[c0 + cc:c0 + cc + m, t0:t0 + 512], o[0:m, :], reads=[bo], writes=[self.b_pT], q="act")

            first = load_chunk(0)
            for t0 in range(0, NT, TA):
                self.norm_tile(objs, l, 1, t0, TA, t0 // S, hT[:, :, t0:t0 + TA], bhs[t0 // TA])
                if (t0 + TA) % 512 == 0:
                    compute(0, *first, t0 + TA - 512)
            for c0 in range(512, N_IN, 512):
                wb, bwb, ncol = load_chunk(c0)
                for t0 in range(0, NT, 512):
                    compute(c0, wb, bwb, ncol, t0)
        P.barrier()

    def phase_B(self, l):
        P, din, NT, S = self.P, self.din, self.NT, self.S
        cb = self.bc
        ST = min(256, S)
        NTL = ST // 128
        with contextlib.ExitStack() as st:
            wo = self.sb(st, [128, 8, D], BF16, "gwo")
            bwo = Buf()
            with contextlib.ExitStack() as st2:
                wst = self.ring(st2, [128, 2, D], F32, 2, "gwst")
                for k2 in range(4):
                    w, bw = wst.next()
                    self.dma(w[:], din["gla_w_o"][l][k2 * 256:(k2 + 1) * 256, :].rearrange("(k p) n -> p k n", p=128), writes=[bw])
                    for kk in range(2):
                        k = k2 * 2 + kk
                        self.act(wo[:, k, :], w[:, kk, :], AF.Identity, [bw, cb], [bwo], scale=self.vec["gla_norm_g"][:, l, k:k + 1])
                P.barrier()
            aup = self.sb(st, [16, 512], F32, "aup")
            baup = Buf()
            self.dma(aup[:], din["gla_alpha_up"][l], writes=[baup])
            ps_a = self.psring(st, [128, 512], F32, 2, "gpsa")
            ps_tk = self.ps(st, [128, 1024], BF16, "gpstk"); bptk = Buf()
            ps_tv, bptv = ps_tk, bptk
            ps_o2 = [self.ps(st, [128, 1024], F32, "gpso") for _ in range(2)]
            ps_s = self.psring(st, [128, 512], F32, 1, "gpss")
            pT = self.pT

            def seq_gen(b):
                ps_o = ps_o2[b]; bpo = Buf()
                S32 = self.sb(st, [128, 4, 256], F32, "S32")
                Sbf = self.sb(st, [128, 4, 256], BF16, "Sbf")
                bS = [Buf() for _ in range(4)]
                stmp = self.ring(st, [128, 256], F32, 2, "stmp")
                q = self.sb(st, [128, 4, ST], F32, "qT"); bq = Buf()
                k_ = self.sb(st, [128, 4, ST], F32, "kT"); bk = Buf()
                v = self.sb(st, [128, 8, ST], F32, "vT"); bv = Buf()
                g = self.sb(st, [128, 8, ST], F32, "gT"); bg = Buf()
                pa = self.sb(st, [16, ST], F32, "paT"); bpa = Buf()
                gar = self.ring(st, [128, ST], F32, 2, "gar")
                lT = self.sb(st, [128, 4, ST], F32, "lT"); blT = Buf()
                cs = self.sb(st, [128, 4, ST], F32, "cs"); bcs = Buf()
                ebT = self.sb(st, [128, 4, ST], F32, "ebT"); beb = Buf()
                enbT = self.sb(st, [128, 4, ST], F32, "enbT"); benb = Buf()
                qeT = self.sb(st, [128, 4, ST], BF16, "qeT"); bqe = Buf()
                keT = self.sb(st, [128, 4, ST], BF16, "keT"); bke = Buf()
                vTb = self.sb(st, [128, 8, ST], BF16, "vTb"); bvb = Buf()
                ofin = self.sb(st, [128, 8, ST], BF16, "ofin"); bof = Buf()
                ketok = self.ring(st, [128, 512], BF16, 2, "ketok")
                vtok = self.ring(st, [128, 1024], BF16, 1, "vtok")
                attm = self.ring(st, [128, 512], BF16, 2, "attm")
                sq = self.ring(st, [128, 1024], BF16, 1, "gsq")
                rstd = self.ring(st, [128, 512], F32, 1, "grstd")
                of = self.ring(st, [128, 1024], F32, 1, "gof")
                ost = self.ring(st, [128, ST], F32, 2, "gost")
                for h in range(4):
                    P.op("pool", lambda e, h=h: e.memset(S32[:, h, :], 0.0), (), [bS[h]])
                    P.op("pool", lambda e, h=h: e.memset(Sbf[:, h, :], 0.0), (), [bS[h]])
                for s0 in range(0, S, ST):
                    t0 = b * S + s0
                    self.dma(q[:], pT[0:512, t0:t0 + ST].rearrange("(h p) t -> p h t", p=128), reads=[self.b_pT], writes=[bq])
                    self.dma(k_[:], pT[512:1024, t0:t0 + ST].rearrange("(h p) t -> p h t", p=128), reads=[self.b_pT], writes=[bk])
                    self.dma(v[:], pT[1024:2048, t0:t0 + ST].rearrange("(h p) t -> p h t", p=128), reads=[self.b_pT], writes=[bv])
                    self.dma(g[:], pT[2048:3072, t0:t0 + ST].rearrange("(h p) t -> p h t", p=128), reads=[self.b_pT], writes=[bg])
                    self.dma(pa[:], pT[3072:3088, t0:t0 + ST], reads=[self.b_pT], writes=[bpa])
                    yield
                    for h in range(4):
                        ps, bps = ps_a.next()
                        self.mm(ps[:, 0:ST], aup[:, h * 128:(h + 1) * 128], pa[:, :], True, True, [baup, bpa], [bps])
                        self.act(lT[:, h, :], ps[:, 0:ST], AF.Exp, [bps, cb], [blT], bias=self.vec["neg_alpha_b"][:, l, h:h + 1], scale=-1.0)
                        if h % 2:
                            yield
                    self.act(lT[:], lT[:], AF.Ln, [blT, cb], [blT], bias=self.cst[:, 0:1])
                    yield
                    P.op("dve", lambda e: e.tensor_tensor_scan(cs[:].rearrange("p h t -> p (h t)"), self.scanm[:, 0:4 * ST],
                                                               lT[:].rearrange("p h t -> p (h t)"), 0.0, ALU.mult, ALU.add),
                         [blT, cb], [bcs])
                    yield
                    self.act(ebT[:], cs[:], AF.Exp, [bcs], [beb], scale=-1.0 / 16)
                    self.act(enbT[:], cs[:], AF.Exp, [bcs], [benb], scale=1.0 / 16)
                    self.cp("act", vTb[:], v[:], [bv], [bvb])
                    yield
                    self.stt("dve", qeT[:], q[:], 128 ** -0.5, ebT[:], ALU.mult, ALU.mult, [bq, beb], [bqe])
                    self.tt("dve", keT[:], k_[:], enbT[:], ALU.mult, [bk, benb], [bke])
                    self.act(g[:], g[:], AF.Silu, [bg], [bg])
                    sg, bsg = g, bg
                    yield
                    for j in range(NTL):
                        c0 = j * 128
                        kt, bkt = ketok.next()
                        vt, bvt = vtok.next()
                        for h in range(4):
                            self.tr(ps_tk[:, h * 128:(h + 1) * 128], keT[:, h, c0:c0 + 128], self.identb[:], [bke, cb], [bptk])
                        self.cp("act", kt[:], ps_tk[:, 0:512], [bptk], [bkt])
                        for c in range(8):
                            self.tr(ps_tv[:, c * 128:(c + 1) * 128], vTb[:, c, c0:c0 + 128], self.identb[:], [bvb, cb], [bptv])
                        self.cp("dve", vt[:], ps_tv[:], [bptv], [bvt])
                        ps, bps = ps_a.next()
                        for h in range(4):
                            self.mm(ps[:, h * 128:(h + 1) * 128], keT[:, h, c0:c0 + 128], qeT[:, h, c0:c0 + 128], True, True, [bke, bqe], [bps])
                        am, bam = attm.next()
                        self.tt("dve", am[:], ps[:], self.masks[:, 0, :], ALU.mult, [bps, cb], [bam])
                        yield
                        for ch in range(2):
                            r0 = ch * 64
                            for h in range(4):
                                for vc in range(2):
                                    o_ap = ps_o[:, (h * 2 + vc) * 128 + r0:(h * 2 + vc) * 128 + r0 + 64]
                                    self.mm(o_ap, Sbf[:, h, vc * 128:(vc + 1) * 128], qeT[:, h, c0 + r0:c0 + r0 + 64], True, False,
                                            [bS[h], bqe], [bpo])
                                    self.mm(o_ap, vt[r0:r0 + 64, h * 256 + vc * 128:h * 256 + (vc + 1) * 128],
                                            am[r0:r0 + 64, h * 128 + r0:h * 128 + r0 + 64], False, True, [bvt, bam], [bpo])
                            for h in range(4):
                                pss, bpss = ps_s.next()
                                self.mm(pss[:, 0:256], kt[r0:r0 + 64, h * 128:(h + 1) * 128], vt[r0:r0 + 64, h * 256:(h + 1) * 256], True, True,
                                        [bkt, bvt], [bpss])
                                tmp, btmp = stmp.next()
                                self.tt("dve", tmp[:], S32[:, h, :], pss[:, 0:256], ALU.add, [bS[h], bpss], [btmp])
                                dl = ebT[:, h, c0 + r0 + 63:c0 + r0 + 64]
                                self.ts("dve", S32[:, h, :], tmp[:], dl, None, ALU.mult, None, [btmp, beb], [bS[h]])
                                self.act(Sbf[:, h, :], tmp[:], AF.Identity, [btmp, beb], [bS[h]], scale=dl)
                                if h % 2:
                                    yield
                        sqt, bsq = sq.next()
                        self.act(sqt[:], ps_o[:], AF.Square, [bpo], [bsq])
                        yield
                        ps, bps = ps_a.next()
                        for h in range(4):
                            for vc in range(2):
                                self.mm(ps[:, h * 128:(h + 1) * 128], self.ones256[:], sqt[:, (h * 2 + vc) * 128:(h * 2 + vc + 1) * 128],
                                        vc == 0, vc == 1, [bsq, cb], [bps])
                        rs, brs = rstd.next()
                        self.act(rs[:], ps[:], AF.Ln, [bps, cb], [brs], bias=self.cst[:, 2:3])
                        yield
                        self.act(rs[:], rs[:], AF.Exp, [brs], [brs], scale=-0.5)
                        yield
                        oft, boft = of.next()
                        for vc in range(2):
                            self.tt("dve", oft[:].rearrange("p (h v t) -> p h v t", h=4, v=2)[:, :, vc, :],
                                    ps_o[:].rearrange("p (h v t) -> p h v t", h=4, v=2)[:, :, vc, :],
                                    rs[:].rearrange("p (h t) -> p h t", h=4), ALU.mult, [bpo, brs], [boft])
                        yield
                        self.tt("pool", ofin[:, :, c0:c0 + 128], oft[:].rearrange("p (c t) -> p c t", c=8), sg[:, :, c0:c0 + 128], ALU.mult,
                                [boft, bsg], [bof])
                        yield
                    for dc in range(8):
                        ga, bga = gar.next()
                        self.dma(ga[:], pT[GAOFF + dc * 128:GAOFF + (dc + 1) * 128, t0:t0 + ST], reads=[self.b_pT], writes=[bga])
                        self.act(ga[:], ga[:], AF.Sigmoid, [bga], [bga])
                        ps, bps = ps_a.next()
                        for k in range(8):
                            self.mm(ps[:, 0:ST], wo[:, k, dc * 128:(dc + 1) * 128], ofin[:, k, :], k == 0, k == 7, [bwo, bof], [bps])
                        o, bo = ost.next()
                        self.tt("dve", o[:, 0:ST], ps[:, 0:ST], ga[:], ALU.mult, [bps, bga], [bo])
                        self.dma(self.yaT[dc * 128:(dc + 1) * 128, t0:t0 + ST], o[:, 0:ST], reads=[bo], writes=[self.b_yaT], q="act")
                        if dc % 2:
                            yield

            active = [seq_gen(0), seq_gen(1)]
            while active:
                for gen in list(active):
                    try:
                        next(gen)
                    except StopIteration:
                        active.remove(gen)
        P.barrier()

    def phase_C(self, l):
        P, din, NT, S = self.P, self.din, self.NT, self.S
        cb = self.bc
        ST = min(256, S)
        NTL = ST // 128
        NDT = self.neu_dt
        V = self.vec
        with contextlib.ExitStack() as st:
            Xsb = self.sb(st, [128, 512], BF16, "Xsb"); Usb = self.sb(st, [128, 512], BF16, "Usb"); bX, bU = Buf(), Buf()
            P.op("pool", lambda e: e.memset(Xsb[:], 0.0), (), [bX])
            P.op("pool", lambda e: e.memset(Usb[:], 0.0), (), [bU])
            rwo = self.sb(st, [128, 4, D], BF16, "rwo"); wout = self.sb(st, [128, 8, D], BF16, "wout"); bw = Buf()
            w2 = self.sb(st, [64, 512], F32, "w2"); a2 = self.sb(st, [64, 512], F32, "a2")
            g2f = self.sb(st, [128, 512], F32, "g2f"); g2 = self.sb(st, [128, 512], BF16, "g2")
            with contextlib.ExitStack() as st2:
                wst = self.ring(st2, [128, 2, D], F32, 2, "cwst")
                for k2 in range(2):
                    w, bws = wst.next()
                    self.dma(w[:], din["rwkv_w_o"][l][k2 * 256:(k2 + 1) * 256, :].rearrange("(k p) n -> p k n", p=128), writes=[bws])
                    self.cp("pool", rwo[:, k2 * 2:k2 * 2 + 2, :], w[:], [bws], [bw])
                for k2 in range(4):
                    w, bws = wst.next()
                    self.dma(w[:], din["w_out"][l][k2 * 256:(k2 + 1) * 256, :].rearrange("(k p) n -> p k n", p=128), writes=[bws])
                    self.cp("pool", wout[:, k2 * 2:k2 * 2 + 2, :], w[:], [bws], [bw])
                self.dma(w2[:], din["rwkv_w2"][l], writes=[bw])
                self.dma(a2[:], din["rwkv_a2"][l], writes=[bw])
                self.dma(g2f[:], din["rwkv_g2"][l], writes=[bw])
                self.cp("pool", g2[:], g2f[:], [bw], [bw])
                P.barrier()
            H32 = [self.sb(st, [128, 4, 64], F32, "H32") for _ in range(2)]
            Hbf = [self.sb(st, [128, 4, 64], BF16, "Hbf") for _ in range(2)]
            Hbd = [self.sb(st, [128, 4, 128], BF16, "Hbd") for _ in range(2)]
            bH = [Buf(), Buf()]
            htmp = self.sb(st, [128, 4, 64], F32, "htmp"); bht = Buf()
            big = lambda nm, dt=F32: (self.sb(st, [128, 4, ST], dt, nm), Buf())
            W0 = self.sb(st, [128, 4, ST + 1], F32, "W0"); bW0 = Buf()
            W1 = self.sb(st, [128, 4, ST + 1], F32, "W1"); bW1 = Buf()
            W2 = self.sb(st, [128, 4, ST + 1], F32, "W2"); bW2 = Buf()
            rP, kP, vP = W0, W1, W2
            wdP = self.sb(st, [64, ST + 1], F32, "wdP"); adP = self.sb(st, [64, ST + 1], F32, "adP"); gdP = self.sb(st, [128, ST + 1], F32, "gdP")
            bsmP = Buf()
            T0, bT0 = big("T0")
            dif, bdif = T0, bT0
            tA, btA = T0, bT0
            rn, brn = T0, bT0
            r_, br = big("r"); k_, bk = big("k"); v_, bv = big("v")
            wd = self.sb(st, [64, ST], F32, "wd"); ad = self.sb(st, [64, ST], F32, "ad"); gd = self.sb(st, [128, ST], F32, "gd")
            gdb = self.sb(st, [128, ST], BF16, "gdb")
            bsm = Buf()
            sw, bsw = big("sw"); cs, bcs = big("cs"); a_, ba = big("a"); gg, bgg = big("gg")
            E1, bE1 = W0[:, :, 0:ST], bW0
            E2, bE2 = W1[:, :, 0:ST], bW1
            kkn, bkkn = W2[:, :, 0:ST], bW2
            E3, bE3 = big("E3")
            k2_, bk2 = big("k2")
            sqk, bsqk = big("sqk", BF16)
            At, bAt = big("At", BF16); Bt, bBt = big("Bt", BF16); Kt, bKt = big("Kt", BF16); Rt, bRt = big("Rt", BF16)
            vb, bvb = big("vb", BF16)
            bonus, bbon = big("bonus")
            yT, byT = big("yT")
            Vtok = self.ring(st, [128, 512], BF16, 2, "Vtok"); Btok = self.ring(st, [128, 512], BF16, 2, "Btok")
            Ktok = self.ring(st, [128, 512], BF16, 2, "Ktok")
            Vpad = self.sb(st, [128, 8, 128], BF16, "Vpad"); Upad = self.sb(st, [128, 8, 128], BF16, "Upad"); bVp = Buf(); bUp = Buf()
            P.op("pool", lambda e: e.memset(Vpad[:], 0.0), (), [bVp])
            P.op("pool", lambda e: e.memset(Upad[:], 0.0), (), [bUp])
            Nm = self.sb(st, [128, 8, 128], NDT, "Nm"); NTm = self.sb(st, [128, 8, 128], NDT, "NTm")
            Mm = [self.sb(st, [128, 8, 128], NDT, "Mm") for _ in range(2)]
            MTm = [self.sb(st, [128, 8, 128], NDT, "MTm") for _ in range(2)]
            Sm = [self.sb(st, [128, 8, 128], NDT, "Sm") for _ in range(2)] if NDT != F32 else [None, None]
            Sm32 = self.sb(st, [128, 8, 128], F32, "Sm32")
            bN, bNT, bM, bMT, bSm = Buf(), Buf(), [Buf(), Buf()], [Buf(), Buf()], [Buf(), Buf()]
            dbl = lambda nm: ([self.sb(st, [128, 8, 128], BF16, nm) for _ in range(2)], [Buf(), Buf()])
            Zb, bZ = dbl("Zb"); LakT, bLak = dbl("LakT"); LrbT, bLrb = dbl("LrbT"); LrkT, bLrk = dbl("LrkT")

            ybf, bybf = sqk, bsqk
            dd, bdd = sw, bsw
            sq2, bsq2 = vb, bvb
            rs2, brs2 = cs, bcs
            yfin, byfin = At, bAt
            gbr = self.ring(st, [128, ST], F32, 2, "gbr")
            yagr = self.ring(st, [128, ST], F32, 2, "yagr")
            xtr = self.ring(st, [128, ST], F32, 2, "xtr")
            mtmp = self.ring(st, [128, ST], F32, 2, "mtmp")
            merged = self.sb(st, [128, 8, ST], BF16, "merged"); bmg = Buf()
            pA = self.psring(st, [128, 512], F32, 2, "cpA")
            pY = self.ps(st, [128, 512], F32, "cpY"); bpY = Buf()
            pTb = self.ps(st, [128, 1024], BF16, "cpTb"); bpTb = Buf()
            pW = [self.ps(st, [128, 1024], F32, "cpW") for _ in range(2)]
            bpW = [Buf(), Buf()]
            pT_ = self.pT
            if self.debug:
                print("phase C sbuf remaining", self.nc.sbuf_bytes_remaining)

            def wide(i):
                return pW[i], bpW[i]

            for b in range(2):
                P.op("pool", lambda e: e.memset(H32[0][:], 0.0), (), [bH[0]])
                P.op("pool", lambda e: e.memset(Hbf[0][:], 0.0), (), [bH[0]])
                P.op("pool", lambda e: e.memset(Hbd[0][:], 0.0), (), [bH[0]])
                P.op("pool", lambda e: e.memset(Hbd[1][:], 0.0), (), [bH[1]])
                hp = 0
                for s0 in range(0, S, ST):
                    t0 = b * S + s0
                    R0 = ROFF
                    lo = 1 if s0 == 0 else 0

                    def ld(dst3, bdst, rows, r0, nchunk):
                        src = pT_[R0 + r0:R0 + r0 + rows, t0 - 1 + lo:t0 + ST]
                        if nchunk > 1:
                            if lo:
                                P.op("pool", lambda e: e.memset(dst3[:, :, 0:1], 0.0), (), [bdst])
                            self.dma(dst3[:, :, lo:ST + 1], src.rearrange("(c p) t -> p c t", p=128), reads=[self.b_pT], writes=[bdst])
                        else:
                            if lo:
                                P.op("pool", lambda e: e.memset(dst3[0:rows, 0:1], 0.0), (), [bdst])
                            self.dma(dst3[0:rows, lo:ST + 1], src, reads=[self.b_pT], writes=[bdst])
                    ld(rP, bW0, 512, 0, 4); ld(wdP, bsmP, 64, 512, 1); ld(kP, bW1, 512, 576, 4); ld(vP, bW2, 512, 1088, 4)
                    ld(adP, bsmP, 64, 1600, 1); ld(gdP, bsmP, 128, 1664, 1)
                    mu = V["mu"]
                    for src, bsrc, dst, bdst, mi in ((rP, bW0, r_, br, 0), (kP, bW1, k_, bk, 5), (vP, bW2, v_, bv, 9)):
                        self.tt("dve", dif[:], src[:, :, 0:ST], src[:, :, 1:ST + 1], ALU.subtract, [bsrc], [bdif])
                        for c in range(4):
                            self.stt("dve", dst[:, c, :], dif[:, c, :], mu[:, l, mi + c:mi + c + 1], src[:, c, 1:ST + 1], ALU.mult, ALU.add,
                                     [bdif, bsrc, cb], [bdst])
                    for src, dst, rows, mi in ((wdP, wd, 64, 4), (adP, ad, 64, 13), (gdP, gd, 128, 14)):
                        self.tt("dve", dif[0:rows, 0, :], src[0:rows, 0:ST], src[0:rows, 1:ST + 1], ALU.subtract, [bsmP], [bdif])
                        self.stt("dve", dst[0:rows, :], dif[0:rows, 0, :], mu[0:rows, l, mi:mi + 1], src[0:rows, 1:ST + 1], ALU.mult, ALU.add,
                                 [bdif, bsmP, cb], [bsm])
                    self.act(wd[:], wd[:], AF.Tanh, [bsm], [bsm])
                    for c in range(4):
                        ps, bps = pA.next()
                        self.mm(ps[:, 0:ST], w2[:, c * 128:(c + 1) * 128], wd[:, :], True, True, [bw, bsm], [bps])
                        self.act(sw[:, c, :], ps[:, 0:ST], AF.Sigmoid, [bps, cb], [bsw], bias=V["rwkv_w0"][:, l, c:c + 1])
                    for c in range(4):
                        ps, bps = pA.next()
                        self.mm(ps[:, 0:ST], a2[:, c * 128:(c + 1) * 128], ad[:, :], True, True, [bw, bsm], [bps])
                        self.act(a_[:, c, :], ps[:, 0:ST], AF.Sigmoid, [bps, cb], [ba], bias=V["rwkv_a0"][:, l, c:c + 1])
                    self.act(gdb[:], gd[:], AF.Sigmoid, [bsm], [bsm])
                    for c in range(4):
                        ps, bps = pA.next()
                        self.mm(ps[:, 0:ST], g2[:, c * 128:(c + 1) * 128], gdb[:, :], True, True, [bw, bsm], [bps])
                        self.cp("dve", gg[:, c, :], ps[:, 0:ST], [bps], [bgg])
                    P.op("dve", lambda e: e.tensor_tensor_scan(cs[:].rearrange("p h t -> p (h t)"), self.scanm[:, 0:4 * ST],
                                                               sw[:].rearrange("p h t -> p (h t)"), 0.0, ALU.mult, ALU.add),
                         [bsw, cb], [bcs])
                    self.tt("dve", tA[:], cs[:], sw[:], ALU.subtract, [bcs, bsw], [btA])
                    self.act(E1[:], tA[:], AF.Exp, [btA], [bE1], scale=-EXPM05)
                    self.act(E2[:], cs[:], AF.Exp, [bcs], [bE2], scale=EXPM05)
                    self.act(E3[:], cs[:], AF.Exp, [bcs], [bE3], scale=-EXPM05)
                    for c in range(4):
                        self.act(sqk[:, c, :], k_[:, c, :], AF.Square, [bk, cb], [bsqk], scale=V["rwkv_k_k"][:, l, c:c + 1])
                    for c in range(4):
                        ps, bps = pA.next()
                        self.mm(ps[:, 0:ST], self.bones[:, 0, :], sqk[:, c, :], True, True, [bsqk, cb], [bps])
                        self.act(rn[:, c, :], ps[:, 0:ST], AF.Ln, [bps, cb], [brn], bias=self.cst[:, 4:5])
                    self.act(rn[:], rn[:], AF.Exp, [brn], [brn], scale=-0.5)
                    for c in range(4):
                        self.stt("dve", kkn[:, c, :], k_[:, c, :], V["rwkv_k_k"][:, l, c:c + 1], rn[:, c, :], ALU.mult, ALU.mult,
                                 [bk, brn, cb], [bkkn])
                    for c in range(4):
                        self.ts("dve", tA[:, c, :], a_[:, c, :], -1.0, V["rwkv_k_a"][:, l, c:c + 1], ALU.add, ALU.mult, [ba, cb, btA], [btA])
                    self.stt("dve", k2_[:], tA[:], 1.0, k_[:], ALU.add, ALU.mult, [btA, bk], [bk2])
                    self.stt("dve", At[:], kkn[:], -1.0, E1[:], ALU.mult, ALU.mult, [bkkn, bE1], [bAt])
                    self.tt("dve", tA[:], kkn[:], a_[:], ALU.mult, [bkkn, ba, btA], [btA])
                    self.tt("dve", Bt[:], tA[:], E2[:], ALU.mult, [btA, bE2], [bBt])
                    self.tt("dve", Kt[:], k2_[:], E2[:], ALU.mult, [bk2, bE2], [bKt])
                    self.tt("dve", Rt[:], r_[:], E3[:], ALU.mult, [br, bE3], [bRt])
                    self.cp("act", vb[:], v_[:], [bv], [bvb])
                    self.tt("dve", tA[:], r_[:], k2_[:], ALU.mult, [br, bk2, btA], [btA])
                    for c in range(4):
                        self.ts("dve", sqk[:, c, :], tA[:, c, :], V["rwkv_r_k"][:, l, c:c + 1], None, ALU.mult, None, [btA, cb, bsqk], [bsqk])
                    for c in range(4):
                        ps, bps = pA.next()
                        self.mm(ps[:, 0:ST], self.bones[:, 0, :], sqk[:, c, :], True, True, [bsqk, cb], [bps])
                        self.tt("dve", bonus[:, c, :], ps[:, 0:ST], v_[:, c, :], ALU.mult, [bps, bv], [bbon])
                    tiles = {}

                    def prep(j):
                        sl = j % 2
                        c0 = j * 128
                        vt, bvt = Vtok.next(); bt, bbt = Btok.next(); kt, bkt = Ktok.next()
                        tiles[j] = (vt, bvt, bt, bbt, kt, bkt)
                        for (src, bsrc, dst, bdst) in ((vb, bvb, vt, bvt), (Bt, bBt, bt, bbt), (Kt, bKt, kt, bkt)):
                            for c in range(4):
                                self.tr(pTb[:, c * 128:(c + 1) * 128], src[:, c, c0:c0 + 128], self.identb[:], [bsrc, cb], [bpTb])
                            self.cp(self.evac_eng(), dst[:], pTb[:, 0:512], [bpTb], [bdst])
                        yield
                        jobs = ((Bt, bBt, At, bAt, 1, Nm, bN), (At, bAt, Bt, bBt, 2, NTm, bNT), (Kt, bKt, At, bAt, 1, LakT[sl], bLak[sl]),
                                (Bt, bBt, Rt, bRt, 0, LrbT[sl], bLrb[sl]), (Kt, bKt, Rt, bRt, 0, LrkT[sl], bLrk[sl]))
                        for ji, (lt, blt, rt, brt, mi, dst, bdst) in enumerate(jobs):
                            pw, bpw = wide(ji % 2)
                            for hidx in range(8):
                                par, q_ = hidx // 4, hidx % 4
                                rows = slice(par * 64, par * 64 + 64)
                                self.mm(pw[:, hidx * 128:(hidx + 1) * 128], lt[rows, q_, c0:c0 + 128], rt[rows, q_, c0:c0 + 128], True, True,
                                        [blt, brt], [bpw])
                            for half in range(2):
                                self.tt("dve", dst[:, half * 4:half * 4 + 4, :],
                                        pw[:, half * 512:(half + 1) * 512].rearrange("p (h t) -> p h t", h=4),
                                        self.masks[:, mi, :].rearrange("p (h t) -> p h t", h=4), ALU.mult, [bpw, cb], [bdst])
                            yield
                        for half in range(2):
                            self.tt("dve", Sm32[:, half * 4:half * 4 + 4, :], Nm[:, half * 4:half * 4 + 4, :],
                                    self.masks[:, 3, :].rearrange("p (h t) -> p h t", h=4), ALU.add, [bN, cb], [bSm[0]])
                        if NDT == F32:
                            Scur, bScur = Sm32, bSm[0]
                        else:
                            self.cp("act", Sm[0][:], Sm32[:], [bSm[0]], [bSm[0]])
                            Scur, bScur = Sm[0], bSm[0]
                        Mc, bMc, MTc, bMTc = Nm, bN, NTm, bNT
                        for step in range(6):
                            if step > 0:
                                pw, bpw = wide(0)
                                for h in range(8):
                                    self.mm(pw[:, h * 128:(h + 1) * 128], MTc[:, h, :], Scur[:, h, :], True, True, [bMTc, bScur], [bpw])
                                nxt = Sm[step % 2] if NDT != F32 else Sm32
                                bnxt = bSm[step % 2] if NDT != F32 else bSm[0]
                                if NDT == F32:
                                    self.tt("dve", Sm32[:].rearrange("p h t -> p (h t)"), pw[:], Sm32[:].rearrange("p h t -> p (h t)"), ALU.add,
                                            [bpw, bScur], [bnxt])
                                else:
                                    self.tt("dve", Sm32[:].rearrange("p h t -> p (h t)"), pw[:], Sm32[:].rearrange("p h t -> p (h t)"), ALU.add,
                                            [bpw, bSm[0], bSm[1]], [bSm[0], bSm[1]])
                                    self.cp("act", nxt[:], Sm32[:], [bSm[0], bSm[1]], [bnxt])
                                Scur, bScur = nxt, bnxt
                            if step < 5:
                                i = step % 2
                                pw, bpw = wide(1)
                                for h in range(8):
                                    self.mm(pw[:, h * 128:(h + 1) * 128], MTc[:, h, :], Mc[:, h, :], True, True, [bMTc, bMc], [bpw])
                                self.cp("act", Mm[i][:].rearrange("p h t -> p (h t)"), pw[:], [bpw], [bM[i]])
                                pw2, bpw2 = wide(0)
                                for h in range(8):
                                    self.mm(pw2[:, h * 128:(h + 1) * 128], Mc[:, h, :], MTc[:, h, :], True, True, [bMTc, bMc], [bpw2])
                                self.cp("act", MTm[i][:].rearrange("p h t -> p (h t)"), pw2[:], [bpw2], [bMT[i]])
                                Mc, bMc, MTc, bMTc = Mm[i], bM[i], MTm[i], bMT[i]
                            yield
                        self.cp("act", Zb[sl][:], Scur[:], [bScur], [bZ[sl]])

                    def seq(j):
                        nonlocal hp
                        sl = j % 2
                        c0 = j * 128
                        vt, bvt, bt, bbt, kt, bkt = tiles[j]
                        for par in range(2):
                            self.cp("pool", Vpad[:].rearrange("p (q w) c -> p q w c", w=2)[:, :, par, par * 64:(par + 1) * 64],
                                    vt[:].rearrange("p (q w c) -> p q w c", w=2, c=64)[:, :, par, :], [bvt], [bVp])
                        for ch in range(2):
                            rows = slice(ch * 64, ch * 64 + 64)
                            Hc32, Hcb, Hcd, bHc = H32[hp], Hbf[hp], Hbd[hp], bH[hp]
                            Hn32, Hnb, Hnd, bHn = H32[1 - hp], Hbf[1 - hp], Hbd[1 - hp], bH[1 - hp]
                            psx, bpsx = pA.next()
                            for h in range(8):
                                q_, par = h // 2, h % 2
                                hi = par * 4 + q_
                                self.mm(psx[:, h * 64:(h + 1) * 64], At[:, q_, c0:c0 + 128], Hcd[:, q_, par * 64:(par + 1) * 64], True, False, [bAt, bHc], [bpsx])
                                self.mm(psx[:, h * 64:(h + 1) * 64], LakT[sl][:, hi, :], vt[:, h * 64:(h + 1) * 64], False, True, [bLak[sl], bvt], [bpsx])
                            self.cp("act", Xsb[rows, :], psx[rows, :], [bpsx], [bX])
                            yield
                            psu, bpsu = pA.next()
                            for h in range(8):
                                hi = (h % 2) * 4 + h // 2
                                self.mm(psu[:, h * 64:(h + 1) * 64], Zb[sl][rows, hi, :], Xsb[rows, h * 64:(h + 1) * 64], True, True, [bZ[sl], bX], [bpsu])
                            self.cp("dve", Usb[rows, :], psu[rows, :], [bpsu], [bU])
                            for par in range(2):
                                self.cp("dve", Upad[rows].rearrange("p (q w) c -> p q w c", w=2)[:, :, par, par * 64:(par + 1) * 64],
                                        psu[rows, :].rearrange("p (q w c) -> p q w c", w=2, c=64)[:, :, par, :], [bpsu], [bUp])
                            yield
                            psh, bpsh = pA.next()
                            for h in range(8):
                                q_, par = h // 2, h % 2
                                o_ap = psh[:, par * 256 + q_ * 64:par * 256 + q_ * 64 + 64]
                                self.mm(o_ap, bt[rows, q_ * 128:(q_ + 1) * 128], Usb[rows, h * 64:(h + 1) * 64], True, False, [bbt, bU], [bpsh])
                                self.mm(o_ap, kt[rows, q_ * 128:(q_ + 1) * 128], vt[rows, h * 64:(h + 1) * 64], False, True, [bkt, bvt], [bpsh])
                            if ch == 0:
                                psy, bpsy = pY, bpY
                            for q_ in range(4):
                                o_ap = psy[:, q_ * 128 + ch * 64:q_ * 128 + ch * 64 + 64]
                                self.mm(o_ap, Hcd[:, q_, :], Rt[:, q_, c0 + ch * 64:c0 + ch * 64 + 64], True, False, [bHc, bRt], [bpsy])
                                for par in range(2):
                                    h = q_ * 2 + par
                                    hi = par * 4 + q_
                                    self.mm(o_ap, Upad[rows, h, :], LrbT[sl][rows, hi, ch * 64:ch * 64 + 64], False, False, [bUp, bLrb[sl]], [bpsy])
                                    self.mm(o_ap, Vpad[rows, h, :], LrkT[sl][rows, hi, ch * 64:ch * 64 + 64], False, par == 1, [bVp, bLrk[sl]], [bpsy])
                            yield
                            for par in range(2):
                                hr = slice(par * 64, par * 64 + 64)
                                self.tt("dve", htmp[hr].rearrange("p q v -> p (q v)"), Hc32[hr].rearrange("p q v -> p (q v)"),
                                        psh[hr, par * 256:(par + 1) * 256], ALU.add, [bHc, bpsh], [bht])
                            wc = E3[:, :, c0 + ch * 64 + 63:c0 + ch * 64 + 64].to_broadcast([128, 4, 64])
                            for par in range(2):
                                hr = slice(par * 64, par * 64 + 64)
                                self.tt("dve", Hnd[hr, :, par * 64:(par + 1) * 64], htmp[hr],
                                        E3[hr, :, c0 + ch * 64 + 63:c0 + ch * 64 + 64].to_broadcast([64, 4, 64]), ALU.mult, [bht, bE3], [bHn])
                            self.tt("dve", Hn32[:], htmp[:], wc, ALU.mult, [bht, bE3], [bHn])
                            hp = 1 - hp
                            yield
                        self.cp("act", yT[:, :, c0:c0 + 128], psy[:].rearrange("p (q t) -> p q t", q=4), [bpsy], [byT])

                    def drain(*gens):
                        act_ = list(gens)
                        while act_:
                            for g_ in list(act_):
                                try:
                                    next(g_)
                                except StopIteration:
                                    act_.remove(g_)

                    drain(prep(0))
                    for j in range(NTL):
                        if j + 1 < NTL:
                            drain(seq(j), prep(j + 1))
                        else:
                            drain(seq(j))
                    if self.cstop < 6:
                        continue
                    self.cp("act", ybf[:], yT[:], [byT], [bybf])
                    for c in range(4):
                        ps, bps = pA.next()
                        self.mm(ps[:, 0:ST], self.bones[:, 1, :], ybf[:, c, :], True, True, [bybf, cb], [bps])
                        self.tt("dve", dd[:, c, :], yT[:, c, :], ps[:, 0:ST], ALU.subtract, [byT, bps], [bdd])
                    self.act(sq2[:], dd[:], AF.Square, [bdd], [bsq2])
                    for c in range(4):
                        ps, bps = pA.next()
                        self.mm(ps[:, 0:ST], self.bones[:, 1, :], sq2[:, c, :], True, True, [bsq2, cb], [bps])
                        self.act(rs2[:, c, :], ps[:, 0:ST], AF.Ln, [bps, cb], [brs2], bias=self.cst[:, 3:4])
                    self.act(rs2[:], rs2[:], AF.Exp, [brs2], [brs2], scale=-0.5)
                    self.tt("dve", dd[:], dd[:], rs2[:], ALU.mult, [bdd, brs2], [bdd])
                    for c in range(4):
                        self.act(dd[:, c, :], dd[:, c, :], AF.Identity, [bdd, cb], [bdd], bias=V["rwkv_lnx_b"][:, l, c:c + 1],
                                 scale=V["rwkv_lnx_g"][:, l, c:c + 1])
                    self.tt("dve", dd[:], dd[:], bonus[:], ALU.add, [bdd, bbon], [bdd])
                    self.tt("dve", yfin[:], dd[:], gg[:], ALU.mult, [bdd, bgg], [byfin])
                    for dc in range(8):
                        gbt, bgbt = gbr.next()
                        self.dma(gbt[:], pT_[GBOFF + dc * 128:GBOFF + (dc + 1) * 128, t0:t0 + ST], reads=[self.b_pT], writes=[bgbt])
                        yat, byat = yagr.next()
                        self.dma(yat[:], self.yaT[dc * 128:(dc + 1) * 128, t0:t0 + ST], reads=[self.b_yaT], writes=[byat])
                        self.act(gbt[:], gbt[:], AF.Sigmoid, [bgbt], [bgbt])
                        ps, bps = pA.next()
                        for k in range(4):
                            self.mm(ps[:, 0:ST], rwo[:, k, dc * 128:(dc + 1) * 128], yfin[:, k, :], k == 0, k == 3, [bw, byfin], [bps])
                        mt, bmt = mtmp.next()
                        self.tt("dve", mt[:], ps[:, 0:ST], gbt[:], ALU.mult, [bps, bgbt], [bmt])
                        self.tt("dve", merged[:, dc, :], mt[:], yat[:], ALU.add, [bmt, byat], [bmg])
                    for dc in range(8):
                        xt, bxt = xtr.next()
                        self.dma(xt[:], self.xT[dc * 128:(dc + 1) * 128, t0:t0 + ST], reads=[self.b_xT], writes=[bxt])
                        ps, bps = pA.next()
                        for k in range(8):
                            self.mm(ps[:, 0:ST], wout[:, k, dc * 128:(dc + 1) * 128], merged[:, k, :], k == 0, k == 7, [bw, bmg], [bps])
                        self.stt("dve", xt[:], ps[:, 0:ST], self.modT[:, l, 16 + dc, b:b + 1], xt[:], ALU.mult, ALU.add,
                                 [bps, cb, bxt], [bxt])
                        self.dma(self.xT[dc * 128:(dc + 1) * 128, t0:t0 + ST], xt[:], reads=[bxt], writes=[self.b_xT], q="act")
        P.barrier()

    def phase_F(self, l):
        P, din, NT, S = self.P, self.din, self.NT, self.S
        cb = self.bc
        moe = (l % 2 == 1)
        li = l // 2
        FD = MOE_FFN if moe else FFN_DIM
        nexp = NEXP if moe else 1
        TB = min(1024, NT)
        TA = min(512, S)
        G = 4
        for p0 in range(0, NT, TB):
            with contextlib.ExitStack() as st:
                hT = self.sb(st, [128, 8, TB], BF16, "fh"); bh = Buf()
                acc = self.sb(st, [128, 8, TB], F32, "facc"); bacc = [Buf() for _ in range(TB // 512)]
                combT = self.sb(st, [8, TB], F32, "combT"); bcomb = Buf()
                cber = self.ring(st, [128, TB], F32, 2, "cbe")
                cbe, bcbe = cber.next()
                with contextlib.ExitStack() as st2:
                    objs = self.norm_objs(st2, TA)
                    if moe:
                        h32 = self.sb(st2, [128, 8, TA], F32, "h32"); bh32 = Buf()
                        rt = self.sb(st2, [128, 8, NEXP], F32, "rt"); brt = Buf()
                        self.dma(rt[:], din["moe_router"][li].rearrange("(k p) e -> p k e", p=128), writes=[brt])
                        psl = self.ps(st2, [128, 512], F32, "psl"); bpsl = Buf()
                        lg = self.sb(st2, [128, 8], F32, "lg"); m8 = self.sb(st2, [128, 8], F32, "m8"); nm1 = self.sb(st2, [128, 1], F32, "nm1")
                        msk = self.sb(st2, [128, 8], F32, "msk"); ex = self.sb(st2, [128, 8], F32, "ex"); ssum = self.sb(st2, [128, 1], F32, "ssum")
                        cmb = self.sb(st2, [128, 8], F32, "cmb")
                        brl = Buf()
                    for t0 in range(p0, p0 + TB, TA):
                        if moe:
                            self.norm_tile(objs, l, 2, t0, TA, t0 // S, hT[:, :, t0 - p0:t0 - p0 + TA], bh, h32, bh32)
                            for j in range(TA // 128):
                                for k in range(8):
                                    self.mm(psl[:, 0:8], h32[:, k, j * 128:(j + 1) * 128], rt[:, k, :], k == 0, k == 7, [bh32, brt], [bpsl])
                                self.cp("dve", lg[:], psl[:, 0:8], [bpsl], [brl])
                                P.op("dve", lambda e: e.max(m8[:], lg[:]), [brl], [brl])
                                self.ts("dve", nm1[:], m8[:, 0:1], -1.0, None, ALU.mult, None, [brl], [brl])
                                self.ts("dve", msk[:], lg[:], m8[:, 1:2], None, ALU.is_ge, None, [brl], [brl])
                                self.act(ex[:], lg[:], AF.Exp, [brl], [brl], bias=nm1[:, 0:1])
                                self.tt("dve", ex[:], ex[:], msk[:], ALU.mult, [brl], [brl])
                                P.op("dve", lambda e: e.tensor_reduce(ssum[:], ex[:], mybir.AxisListType.X, ALU.add), [brl], [brl])
                                P.op("dve", lambda e: e.reciprocal(ssum[:], ssum[:]), [brl], [brl])
                                self.ts("dve", cmb[:], ex[:], ssum[:, 0:1], None, ALU.mult, None, [brl], [brl])
                                self.tr(psl[0:8, 128:256], cmb[:], self.identf[:], [brl, cb], [bpsl])
                                self.cp("dve", combT[:, t0 - p0 + j * 128:t0 - p0 + (j + 1) * 128], psl[0:8, 128:256], [bpsl], [bcomb])
                        else:
                            self.norm_tile(objs, l, 2, t0, TA, t0 // S, hT[:, :, t0 - p0:t0 - p0 + TA], bh)
                    P.barrier()
                P.op("pool", lambda e: e.memset(acc[:], 0.0), (), bacc)
                wst = self.ring(st, [128, 8, 256], F32, 5, "fwst")
                w1b = self.ring(st, [128, 8, G * 128], BF16, 2, "fw1")
                w3b = self.ring(st, [128, 8, G * 128], BF16, 2, "fw3")
                w2b = self.ring(st, [128, G, D], BF16, 3, "fw2")
                gp = self.ring(st, [128, G, 512], BF16, 3, "fgp")
                sil = self.ring(st, [128, 512], F32, 2, "fsil")
                tq = self.ring(st, [128, 512], F32, 2, "ftq")
                ps1 = self.psring(st, [128, 512], F32, 2, "fps1")
                ps3 = self.psring(st, [128, 512], F32, 2, "fps3")
                psy = self.psring(st, [128, 512], F32, 3, "fpsy")
                pscb = self.ps(st, [128, 512], F32, "fpscb"); bpscb = Buf()
                ce = 0
                pendY = []

                def emit_Y(a2, ba2, g_, bg_, ng, tt0, ti):
                    for dc in range(8):
                        py, bpy = psy.next()
                        for fi in range(ng):
                            self.mm(py[:], a2[:, fi, dc * 128:(dc + 1) * 128], g_[:, fi, :], fi == 0, fi == ng - 1, [ba2, bg_], [bpy])
                        self.tt("dve", acc[:, dc, tt0:tt0 + 512], py[:], acc[:, dc, tt0:tt0 + 512], ALU.add, [bpy, bacc[ti]], [bacc[ti]])

                groups = []
                for e_ in range(nexp):
                    for f0 in range(0, FD, G * 128):
                        groups.append((e_, f0))
                loaded = {}
                cbes = {}
                cnt_ce = [0]

                def load_group(gi):
                    e_, f0 = groups[gi]
                    if moe:
                        W1, W3, W2 = din["moe_w1"][li * NEXP + e_], din["moe_w3"][li * NEXP + e_], din["moe_w2"][li * NEXP + e_]
                    else:
                        W1, W3, W2 = din["ffn_w1"][li], din["ffn_w3"][li], din["ffn_w2"][li]
                    nf = min(G * 128, FD - f0)
                    ng = nf // 128
                    a1, ba1 = w1b.next(); a3, ba3 = w3b.next(); a2, ba2 = w2b.next()
                    for (Wsrc, dstt, bdst) in ((W1, a1, ba1), (W3, a3, ba3)):
                        for hf in range(0, nf, 256):
                            w, bws = wst.next()
                            cnt_ce[0] += 1
                            self.dma(w[:], Wsrc[:, f0 + hf:f0 + hf + 256].rearrange("(k p) n -> p k n", p=128), writes=[bws],
                                     q="sp" if cnt_ce[0] % 2 else "pool")
                            self.cp("act", dstt[:, :, hf:hf + 256], w[:], [bws], [bdst])
                    for hf in range(0, ng, 2):
                        w, bws = wst.next()
                        wv = w[:].rearrange("p k n -> p (k n)").rearrange("p (f d) -> p f d", f=2)
                        cnt_ce[0] += 1
                        self.dma(wv, W2[f0 + hf * 128:f0 + (hf + 2) * 128, :].rearrange("(f p) d -> p f d", p=128), writes=[bws],
                                 q="sp" if cnt_ce[0] % 2 else "pool")
                        self.cp("act", a2[:, hf:hf + 2, :], wv, [bws], [ba2])
                    loaded[gi] = (a1, ba1, a3, ba3, a2, ba2, ng)
                    if moe and e_ not in cbes:
                        cbe, bcbe = cber.next()
                        for tt0 in range(0, TB, 512):
                            self.mm(pscb[:], self.sel[:, e_, :], combT[:, tt0:tt0 + 512], True, True, [bcomb, cb], [bpscb])
                            self.cp("act", cbe[:, tt0:tt0 + 512], pscb[:], [bpscb], [bcbe])
                        cbes[e_] = (cbe, bcbe)

                load_group(0)
                for gi, (e_, f0) in enumerate(groups):
                    if gi + 1 < len(groups):
                        load_group(gi + 1)
                    a1, ba1, a3, ba3, a2, ba2, ng = loaded.pop(gi)
                    if moe:
                        cbe, bcbe = cbes[e_]
                    if True:
                        for ti, tt0 in enumerate(range(0, TB, 512)):
                            g_, bg_ = gp.next()
                            for fi in range(ng):
                                p1, bp1 = ps1.next(); p3, bp3 = ps3.next()
                                for k in range(8):
                                    self.mm(p1[:], a1[:, k, fi * 128:(fi + 1) * 128], hT[:, k, tt0:tt0 + 512], k == 0, k == 7, [ba1, bh], [bp1])
                                for k in range(8):
                                    self.mm(p3[:], a3[:, k, fi * 128:(fi + 1) * 128], hT[:, k, tt0:tt0 + 512], k == 0, k == 7, [ba3, bh], [bp3])
                                s_, bs_ = sil.next()
                                self.act(s_[:], p1[:], AF.Silu, [bp1], [bs_])
                                if moe:
                                    q_, bq_ = tq.next()
                                    self.tt("dve", q_[:], p3[:], s_[:], ALU.mult, [bp3, bs_], [bq_])
                                    self.tt("dve", g_[:, fi, :], q_[:], cbe[:, tt0:tt0 + 512], ALU.mult, [bq_, bcbe], [bg_])
                                else:
                                    self.tt("dve", g_[:, fi, :], p3[:], s_[:], ALU.mult, [bp3, bs_], [bg_])
                            if pendY:
                                emit_Y(*pendY.pop())
                            pendY.append((a2, ba2, g_, bg_, ng, tt0, ti))
                if pendY:
                    emit_Y(*pendY.pop())
                with contextlib.ExitStack() as st3:
                    xr = self.ring(st3, [128, TA], F32, 3, "fxr")
                    for t0 in range(p0, p0 + TB, TA):
                        b = t0 // S
                        for dc in range(8):
                            xt, bxt = xr.next()
                            self.dma(xt[:], self.xT[dc * 128:(dc + 1) * 128, t0:t0 + TA], reads=[self.b_xT], writes=[bxt])
                            self.stt("dve", xt[:], acc[:, dc, t0 - p0:t0 - p0 + TA], self.modT[:, l, 40 + dc, b:b + 1],
                                     xt[:], ALU.mult, ALU.add, [bacc[(t0 - p0) // 512], cb, bxt], [bxt])
                            self.dma(self.xT[dc * 128:(dc + 1) * 128, t0:t0 + TA], xt[:], reads=[bxt], writes=[self.b_xT], q="act")
            P.barrier()

    def final(self):
        P, NT = self.P, self.NT
        cb = self.bc
        with contextlib.ExitStack() as st:
            xr = self.ring(st, [128, 8, 512], F32, 2, "zx")
            sq = self.sb(st, [128, 8, 512], BF16, "zsq"); bsq = Buf()
            psn = self.ps(st, [128, 512], F32, "zpsn"); bpsn = Buf()
            rstd = self.sb(st, [128, 512], F32, "zrs"); brs = Buf()
            yv = self.sb(st, [128, 8, 512], F32, "zy"); by = Buf()
            pst = self.psring(st, [128, 512], F32, 4, "zpst")
            ot = self.ring(st, [128, D], F32, 3, "zot")
            for t0 in range(0, NT, 512):
                xt, bx = xr.next()
                self.dma(xt[:], self.xT[:, t0:t0 + 512].rearrange("(c p) t -> p c t", p=128), reads=[self.b_xT], writes=[bx])
                self.act(sq[:], xt[:], AF.Square, [bx], [bsq])
                for k in range(8):
                    self.mm(psn[:], self.onesD[:], sq[:, k, :], k == 0, k == 7, [bsq, cb], [bpsn])
                self.act(rstd[:], psn[:], AF.Ln, [bpsn, cb], [brs], bias=self.cst[:, 1:2])
                self.act(rstd[:], rstd[:], AF.Exp, [brs], [brs], scale=-0.5)
                for k in range(8):
                    self.stt("dve", yv[:, k, :], xt[:, k, :], self.vec["lnf_g"][:, 0, k:k + 1], rstd[:], ALU.mult, ALU.mult,
                             [bx, brs, cb], [by])
                for j in range(4):
                    o, bo = ot.next()
                    for half in range(2):
                        ps, bps = pst.next()
                        for q in range(4):
                            k = half * 4 + q
                            self.tr(ps[:, q * 128:(q + 1) * 128], yv[:, k, j * 128:(j + 1) * 128], self.identf[:], [by, cb], [bps])
                        self.cp(self.evac_eng(), o[:, half * 512:(half + 1) * 512], ps[:], [bps], [bo])
                    self.dma(self.out[t0 + j * 128:t0 + (j + 1) * 128, :], o[:], reads=[bo], q="act")
        P.barrier()

    def build(self, do_mixer=True, do_ffn=True, phases="ABC"):
        self.declare()
        with contextlib.ExitStack() as st:
            self.setup_consts(st)
            self.prologue()
            for l in range(self.L):
                if do_mixer:
                    if "A" in phases:
                        self.phase_A(l)
                    if "B" in phases:
                        self.phase_B(l)
                    if "C" in phases:
                        self.phase_C(l)
                if do_ffn:
                    self.phase_F(l)
            self.final()
        self.P.emit()
        return self.nc


def host_consts():
    ident = np.eye(128, dtype=np.float32)
    s = np.arange(128)[:, None]
    t = np.arange(128)[None, :]
    same = (s // 64) == (t // 64)
    m_iu = (same & (s <= t)).astype(np.float32)
    m_su = (same & (s < t)).astype(np.float32)
    m_sl = (same & (s > t)).astype(np.float32)
    masks = np.zeros((128, 5, 512), np.float32)
    for i, m in enumerate([m_iu, m_su, m_sl, ident]):
        masks[:, i, :] = np.tile(m, (1, 4))
    bones = np.zeros((128, 2, 128), np.float32)
    blk = ((s // 64) == (t // 64)).astype(np.float32)
    bones[:, 0, :] = blk
    bones[:, 1, :] = blk / 64.0
    sel = np.zeros((8, 8, 128), np.float32)
    for e in range(8):
        sel[e, e, :] = 1.0
    scanm = np.ones((128, 1024), np.float32)
    scanm[:, ::64] = 0.0
    return dict(k_ident=ident, k_masks=masks, k_bones=bones, k_sel=sel, k_scanm=scanm)


_CACHE = {}


def run(inputs, S, L, n_cores, **bk):
    key = (S, L, tuple(sorted(bk.items())))
    if key not in _CACHE:
        kb = KB(S, L, neu_fp32=bk.pop("neu_fp32", True), debug=bk.pop("debug", False))
        kb.cstop = bk.pop("cstop", 9)
        _CACHE[key] = kb.build(**bk)
    nc = _CACHE[key]
    hc = host_consts()
    x = np.ascontiguousarray(inputs["x"], dtype=np.float32)
    in_maps = []
    shared = {}
    for k, v in inputs.items():
        if k in ("x", "c"):
            continue
        a = np.ascontiguousarray(v, dtype=np.float32)
        if k in ("moe_w1", "moe_w3", "moe_w2"):
            a = a.reshape((-1,) + a.shape[2:])
        if k == "rwkv_r_k":
            a = a.reshape(a.shape[0], 512)
        shared[k] = a
    shared.update(hc)
    for i in range(n_cores):
        m = dict(shared)
        m["x"] = x[2 * i:2 * i + 2].reshape(2 * S, D)
        m["c"] = np.ascontiguousarray(inputs["c"][2 * i:2 * i + 2], dtype=np.float32)
        in_maps.append(m)
    import os as _os
    if _os.environ.get("KTRACE"):
        res = run_bass_kernel_spmd(nc, in_maps, core_ids=list(range(n_cores)), trace=True)
        print("EXEC_TIME_NS", res.exec_time_ns)
    else:
        res = run_bass_kernel_spmd(nc, in_maps, core_ids=list(range(n_cores)))
    global LAST
    LAST = res.results
    out = np.stack([r["out"].reshape(2, S, D) for r in res.results], axis=0)
    return out.reshape(2 * n_cores, S, D).astype(np.float32)


def kernel(**inputs):
    return run(inputs, 2048, 4, 8)
```
